# Optimizing a Trainium2 kernel written in Bass

```python
import jax
import jax.numpy as jnp
from jax import lax
import numpy as np

D_MODEL = 2048
BATCH = 8
SEQ = 2048
DEPTH = 1

GRID_W = 64
N_META = 16
NA_HEADS = 16
NA_HEAD_DIM = 64
NA_WIN_H_MAX = 8
NA_WIN_W = 16
NA_QBLOCK_W = NA_WIN_W
NA_KBLOCK_W = 2 * NA_WIN_W
MLA_HEADS = 16
MLA_Q_RANK = 512
MLA_KV_RANK = 512
MLA_NOPE_DIM = 128
MLA_ROPE_DIM = 64
MLA_V_DIM = 128
MLA_QBLOCK = 128
ROPE_THETA = 10000.0
PEER_HEADS = 8
PEER_NKEYS = 128
PEER_EXPERTS = PEER_NKEYS * PEER_NKEYS
PEER_DK = 256
PEER_TOPK = 16
PEER_TOKEN_CHUNK = 16
NORM_EPS = 1e-6
NEG_INF = -1e30

NA_WIDTH = NA_HEADS * NA_HEAD_DIM
MLA_Q_WIDTH = MLA_HEADS * (MLA_NOPE_DIM + MLA_ROPE_DIM)
MLA_KV_WIDTH = MLA_HEADS * (MLA_NOPE_DIM + MLA_V_DIM)
MLA_OUT_WIDTH = MLA_HEADS * MLA_V_DIM
IN_SIZES = (NA_WIDTH, NA_WIDTH, NA_WIDTH, MLA_Q_RANK, MLA_KV_RANK, MLA_ROPE_DIM, D_MODEL, D_MODEL)
IN_WIDTH = sum(IN_SIZES)

kernel_name = 'hybrid_na_mla_peer_block'


def rmsnorm(x, g):
    xf = x.astype(jnp.float32)
    y = xf * lax.rsqrt(jnp.mean(xf * xf, axis=-1, keepdims=True) + NORM_EPS)
    return (y * g.astype(jnp.float32)).astype(x.dtype)


def rope(x, cos, sin):
    half = x.shape[-1] // 2
    x1 = x[..., :half].astype(jnp.float32)
    x2 = x[..., half:].astype(jnp.float32)
    return jnp.concatenate([x1 * cos - x2 * sin, x2 * cos + x1 * sin], axis=-1).astype(x.dtype)


def _na_static(rows):
    wh = min(NA_WIN_H_MAX, rows)
    r = np.arange(rows)
    row_start = np.clip(r - wh // 2, 0, rows - wh)
    row_off = row_start[:, None] + np.arange(wh)[None] - r[:, None] + (NA_WIN_H_MAX - 1)
    ncb = GRID_W // NA_QBLOCK_W
    qcol = np.arange(ncb)[:, None] * NA_QBLOCK_W + np.arange(NA_QBLOCK_W)[None]
    kcol0 = np.clip(np.arange(ncb) * NA_QBLOCK_W - NA_WIN_W // 2, 0, GRID_W - NA_KBLOCK_W)
    kcol = kcol0[:, None] + np.arange(NA_KBLOCK_W)[None]
    cstart = np.clip(qcol - NA_WIN_W // 2, 0, GRID_W - NA_WIN_W)
    kc = kcol[:, None, :]
    col_mask = (kc >= cstart[:, :, None]) & (kc < cstart[:, :, None] + NA_WIN_W)
    col_off = np.clip(kc - qcol[:, :, None], -(NA_WIN_W - 1), NA_WIN_W - 1) + NA_WIN_W - 1
    return wh, row_start.astype(np.int32), row_off, kcol, col_mask, col_off


def neighbourhood_attention(q, k, v, rel_bias, meta_bias):
    b, t, h, dh = q.shape
    n = t - N_META
    rows = n // GRID_W
    wh, row_start, row_off, kcol, col_mask, col_off = _na_static(rows)
    ncb = GRID_W // NA_QBLOCK_W
    scale = dh ** -0.5
    qm, km, vm = q[:, :N_META], k[:, :N_META], v[:, :N_META]
    qg = q[:, N_META:].reshape(b, rows, GRID_W, h, dh)
    kg = k[:, N_META:].reshape(b, rows, GRID_W, h, dh)
    vg = v[:, N_META:].reshape(b, rows, GRID_W, h, dh)
    mb = meta_bias.astype(jnp.float32)
    sm = jnp.einsum('bqhd,bkhd->bhqk', qm, km).astype(jnp.float32) * scale + mb[None, :, None, :]
    om = jnp.einsum('bhqk,bkhd->bqhd', jax.nn.softmax(sm, axis=-1).astype(v.dtype), vm)
    bias = rel_bias.astype(jnp.float32)[:, row_off[:, :, None, None, None], col_off[None, None]]
    bias = bias.transpose(1, 0, 3, 4, 2, 5)
    bias = jnp.where(jnp.asarray(col_mask)[None, None, :, :, None, :], bias, NEG_INF)
    q_rows = qg.reshape(b, rows, ncb, NA_QBLOCK_W, h, dh).transpose(1, 0, 2, 3, 4, 5)
    n_loc = wh * NA_KBLOCK_W

    def row_fn(args):
        q_r, rs, bias_r = args
        k_blk = lax.dynamic_slice_in_dim(kg, rs, wh, axis=1)[:, :, kcol]
        v_blk = lax.dynamic_slice_in_dim(vg, rs, wh, axis=1)[:, :, kcol]
        s_loc = jnp.einsum('bjqhd,bwjkhd->bhjqwk', q_r, k_blk).astype(jnp.float32) * scale + bias_r[None]
        s_meta = jnp.einsum('bjqhd,bmhd->bhjqm', q_r, km).astype(jnp.float32) * scale + mb[None, :, None, None, :]
        s = jnp.concatenate([s_loc.reshape(b, h, ncb, NA_QBLOCK_W, n_loc), s_meta], axis=-1)
        p = jax.nn.softmax(s, axis=-1).astype(v.dtype)
        p_loc = p[..., :n_loc].reshape(b, h, ncb, NA_QBLOCK_W, wh, NA_KBLOCK_W)
        return (jnp.einsum('bhjqwk,bwjkhd->bjqhd', p_loc, v_blk)
                + jnp.einsum('bhjqm,bmhd->bjqhd', p[..., n_loc:], vm))

    o_rows = lax.map(row_fn, (q_rows, jnp.asarray(row_start), bias))
    og = o_rows.transpose(1, 0, 2, 3, 4, 5).reshape(b, n, h, dh)
    return jnp.concatenate([om, og], axis=1)


def latent_attention(c_q, c_kv, k_rope_raw, q_norm_g, w_uq, kv_norm_g, w_ukv, cos, sin):
    b, t, _ = c_q.shape
    q = (rmsnorm(c_q, q_norm_g) @ w_uq).reshape(b, t, MLA_HEADS, MLA_NOPE_DIM + MLA_ROPE_DIM)
    q_nope = q[..., :MLA_NOPE_DIM]
    q_rope = rope(q[..., MLA_NOPE_DIM:], cos[:, None, :], sin[:, None, :])
    kv = (rmsnorm(c_kv, kv_norm_g) @ w_ukv).reshape(b, t, MLA_HEADS, MLA_NOPE_DIM + MLA_V_DIM)
    k_nope, val = kv[..., :MLA_NOPE_DIM], kv[..., MLA_NOPE_DIM:]
    k_rope = rope(k_rope_raw, cos, sin)
    scale = (MLA_NOPE_DIM + MLA_ROPE_DIM) ** -0.5

    def attend(qn, qr):
        s = (jnp.einsum('bqhd,bkhd->bhqk', qn, k_nope)
             + jnp.einsum('bqhr,bkr->bhqk', qr, k_rope)).astype(jnp.float32) * scale
        p = jax.nn.softmax(s, axis=-1).astype(val.dtype)
        return jnp.einsum('bhqk,bkhd->bqhd', p, val)

    o_meta = attend(q_nope[:, :N_META], q_rope[:, :N_META])
    n = t - N_META
    nb = n // MLA_QBLOCK

    def blocks(a):
        return a[:, N_META:].reshape((b, nb, MLA_QBLOCK) + a.shape[2:]).swapaxes(0, 1)

    o_real = lax.map(lambda a: attend(a[0], a[1]), (blocks(q_nope), blocks(q_rope)))
    o_real = o_real.swapaxes(0, 1).reshape(b, n, MLA_HEADS, MLA_V_DIM)
    return jnp.concatenate([o_meta, o_real], axis=1).reshape(b, t, MLA_OUT_WIDTH)


def peer(hn, w_q, sub_keys, u, v):
    b, t, d = hn.shape
    q = (hn @ w_q).reshape(b, t, PEER_HEADS, 2, PEER_DK // 2)
    s = jnp.einsum('bthsc,hsnc->bthsn', q, sub_keys).astype(jnp.float32)
    s_top, i_top = lax.top_k(s, PEER_TOPK)
    cand = s_top[..., 0, :, None] + s_top[..., 1, None, :]
    c_top, c_idx = lax.top_k(cand.reshape(b, t, PEER_HEADS, PEER_TOPK * PEER_TOPK), PEER_TOPK)
    i1 = jnp.take_along_axis(i_top[..., 0, :], c_idx // PEER_TOPK, axis=-1)
    i2 = jnp.take_along_axis(i_top[..., 1, :], c_idx % PEER_TOPK, axis=-1)
    expert = i1 * PEER_NKEYS + i2
    g = jax.nn.softmax(c_top, axis=-1).astype(hn.dtype)
    nc = (b * t) // PEER_TOKEN_CHUNK
    hf = hn.reshape(nc, PEER_TOKEN_CHUNK, d)
    ef = expert.reshape(nc, PEER_TOKEN_CHUNK, PEER_HEADS * PEER_TOPK)
    gf = g.reshape(nc, PEER_TOKEN_CHUNK, PEER_HEADS * PEER_TOPK)

    def chunk_fn(a):
        h_c, e_c, g_c = a
        act = jax.nn.gelu(jnp.einsum('nd,nkd->nk', h_c, u[e_c]))
        return jnp.einsum('nk,nkd->nd', g_c * act, v[e_c])

    return lax.map(chunk_fn, (hf, ef, gf)).reshape(b, t, d)


def setup_inputs(seed: int = 0) -> dict:
    key = jax.random.key(seed)
    ks = jax.random.split(key, 19)

    def nrm(k, shape, scale):
        return jax.random.normal(k, shape, jnp.float32) * scale

    def gain(k, shape):
        return 1.0 + 0.02 * jax.random.normal(k, shape, jnp.float32)

    return {
        'x': nrm(ks[0], (BATCH, SEQ, D_MODEL), 1.0),
        'meta_tokens': nrm(ks[1], (N_META, D_MODEL), 1.0),
        'norm1_g': gain(ks[2], (DEPTH, D_MODEL)),
        'w_in': nrm(ks[3], (DEPTH, D_MODEL, IN_WIDTH), D_MODEL ** -0.5),
        'na_rel_bias': nrm(ks[4], (DEPTH, NA_HEADS, 2 * NA_WIN_H_MAX - 1, 2 * NA_WIN_W - 1), 0.1),
        'na_meta_bias': nrm(ks[5], (DEPTH, NA_HEADS, N_META), 0.1),
        'mla_q_norm_g': gain(ks[6], (DEPTH, MLA_Q_RANK)),
        'mla_w_uq': nrm(ks[7], (DEPTH, MLA_Q_RANK, MLA_Q_WIDTH), MLA_Q_RANK ** -0.5),
        'mla_kv_norm_g': gain(ks[8], (DEPTH, MLA_KV_RANK)),
        'mla_w_ukv': nrm(ks[9], (DEPTH, MLA_KV_RANK, MLA_KV_WIDTH), MLA_KV_RANK ** -0.5),
        'w_na_branch': nrm(ks[10], (DEPTH, NA_WIDTH, D_MODEL), NA_WIDTH ** -0.5),
        'w_mla_branch': nrm(ks[11], (DEPTH, MLA_OUT_WIDTH, D_MODEL), MLA_OUT_WIDTH ** -0.5),
        'w_out': nrm(ks[12], (DEPTH, D_MODEL, D_MODEL), D_MODEL ** -0.5),
        'norm2_g': gain(ks[13], (DEPTH, D_MODEL)),
        'peer_w_q': nrm(ks[14], (DEPTH, D_MODEL, PEER_HEADS * PEER_DK), D_MODEL ** -0.5),
        'peer_sub_keys': nrm(ks[15], (DEPTH, PEER_HEADS, 2, PEER_NKEYS, PEER_DK // 2), (PEER_DK // 2) ** -0.5),
        'peer_u': nrm(ks[16], (DEPTH, PEER_EXPERTS, D_MODEL), D_MODEL ** -0.5),
        'peer_v': nrm(ks[17], (DEPTH, PEER_EXPERTS, D_MODEL), PEER_HEADS ** -0.5),
        'final_norm_g': gain(ks[18], (D_MODEL,)),
    }


def reference(x, meta_tokens, norm1_g, w_in, na_rel_bias, na_meta_bias, mla_q_norm_g, mla_w_uq,
              mla_kv_norm_g, mla_w_ukv, w_na_branch, w_mla_branch, w_out, norm2_g, peer_w_q,
              peer_sub_keys, peer_u, peer_v, final_norm_g):
    b, s, d = x.shape
    t = s + N_META
    h = jnp.concatenate([jnp.broadcast_to(meta_tokens.astype(x.dtype)[None], (b, N_META, d)), x], axis=1)
    pos = jnp.arange(t, dtype=jnp.float32)
    inv_freq = ROPE_THETA ** (-jnp.arange(0, MLA_ROPE_DIM, 2, dtype=jnp.float32) / MLA_ROPE_DIM)
    ang = pos[:, None] * inv_freq[None, :]
    cos, sin = jnp.cos(ang), jnp.sin(ang)
    offsets = [int(o) for o in np.cumsum(IN_SIZES)[:-1]]
    for l in range(DEPTH):
        hn = rmsnorm(h, norm1_g[l])
        na_q, na_k, na_v, c_q, c_kv, k_rope_raw, gate_a, gate_b = jnp.split(hn @ w_in[l], offsets, axis=-1)
        hd = (b, t, NA_HEADS, NA_HEAD_DIM)
        o_na = neighbourhood_attention(na_q.reshape(hd), na_k.reshape(hd), na_v.reshape(hd),
                                       na_rel_bias[l], na_meta_bias[l]).reshape(b, t, NA_WIDTH)
        o_mla = latent_attention(c_q, c_kv, k_rope_raw, mla_q_norm_g[l], mla_w_uq[l],
                                 mla_kv_norm_g[l], mla_w_ukv[l], cos, sin)
        merged = (jax.nn.sigmoid(gate_a) * (o_na @ w_na_branch[l])
                  + jax.nn.sigmoid(gate_b) * (o_mla @ w_mla_branch[l]))
        h = h + merged @ w_out[l]
        h = h + peer(rmsnorm(h, norm2_g[l]), peer_w_q[l], peer_sub_keys[l], peer_u[l], peer_v[l])
    return rmsnorm(h[:, N_META:], final_norm_g)
```

```python
import numpy as np
from contextlib import ExitStack
import concourse.bass as bass
import concourse.mybir as mybir
from concourse.bass_utils import run_bass_kernel_spmd

F32 = mybir.dt.float32
BF16 = mybir.dt.bfloat16
I32 = mybir.dt.int32
U32 = mybir.dt.uint32
AF = mybir.ActivationFunctionType
ALU = mybir.AluOpType
AX = mybir.AxisListType

D = 2048
S = 2048
NM = 16
T = S + NM
EPS = 1e-6
NCORES = 8
QB = [(i * 512, 512) for i in range(4)]
TB = QB + [(2048, 16)]
NA_SCALE = 64 ** -0.5
MLA_SCALE = 192 ** -0.5
NEG = -30000.0


class Res:
    __slots__ = ("lw", "rd")

    def __init__(self):
        self.lw = None
        self.rd = {}


class Prog:
    ENG = ("pe", "act", "dve", "pool", "sp")

    def __init__(self, nc, stack):
        self.nc = nc
        self.stack = stack
        self.sem = {}
        self.val = {}
        self.seen = {e: {} for e in self.ENG}
        self.q = {e: [] for e in self.ENG}
        self.n = 0

    def _sem(self, key):
        if key not in self.sem:
            self.sem[key] = self.stack.enter_context(self.nc.semaphore("s_" + key))
            self.val[key] = 0

    def _waits(self, eng, reads, writes):
        need = {}

        def add(k, v):
            if need.get(k, 0) < v:
                need[k] = v

        for r in reads:
            if r.lw is not None:
                add(*r.lw)
        for w in writes:
            if w.lw is not None:
                add(*w.lw)
            for k, v in w.rd.items():
                add(k, v)
        out = []
        for k, v in need.items():
            if k == "pe" and eng == "pe":
                continue
            if self.seen[eng].get(k, 0) < v:
                self.seen[eng][k] = v
                out.append((k, v))
        return out

    def op(self, eng, fns, reads=(), writes=(), semkey=None, inc=1):
        if not isinstance(fns, (list, tuple)):
            fns = [fns]
        waits = self._waits(eng, reads, writes)
        key = semkey or eng
        self._sem(key)
        self.val[key] += inc
        ev = (key, self.val[key])
        for r in reads:
            if r.rd.get(key, 0) < ev[1]:
                r.rd[key] = ev[1]
        for w in writes:
            w.lw = ev
            w.rd = {}
        self.q[eng].append((waits, fns, key, inc))
        self.n += len(fns)
        return ev

    def dma(self, eng, out, in_, reads, writes, semkey, **kw):
        return self.op(eng, lambda e: e.dma_start(out=out, in_=in_, **kw), reads, writes, semkey=semkey, inc=16)

    def flush(self, drain=True):
        nc = self.nc
        if drain:
            w = []
            for k, v in self.val.items():
                if k not in self.ENG and self.seen["sp"].get(k, 0) < v:
                    self.seen["sp"][k] = v
                    w.append((k, v))
            if w:
                self.q["sp"].append((w, [], None, 0))
        sem = self.sem
        with nc.Block() as block:
            for ename, deco in (("pe", block.tensor), ("act", block.scalar), ("dve", block.vector),
                                ("pool", block.gpsimd), ("sp", block.sync)):
                items = self.q[ename]

                def body(e, items=items):
                    for waits, fns, key, inc in items:
                        for k, v in waits:
                            e.wait_ge(sem[k], v)
                        last = None
                        for fn in fns:
                            last = fn(e)
                        if last is not None:
                            last.then_inc(sem[key], inc)

                deco(body)
        self.q = {e: [] for e in self.ENG}


class Ctx:
    pass


def _sb(nc, st, name, shape, dt):
    return st.enter_context(nc.sbuf_tensor(name, list(shape), dt))


def _ps(nc, st, name, shape, dt):
    return st.enter_context(nc.psum_tensor(name, list(shape), dt))


def build(debug=None, stop_after=None):
    debug = debug or ()
    nc = bass.Bass("TRN2", target_bir_lowering=False)

    def din(name, shape, dt=F32):
        return nc.dram_tensor(name, list(shape), dt, kind="ExternalInput").ap()

    def dscr(name, shape, dt):
        kind = "ExternalOutput" if name in debug else "Internal"
        return nc.dram_tensor(name, list(shape), dt, kind=kind).ap()

    x = din("x", [S, D])
    meta = din("meta", [NM, D])
    g1 = din("g1", [1, D])
    g2 = din("g2", [1, D])
    gf = din("gf", [1, D])
    w_in = din("w_in", [D, 8256])
    w_kr2 = din("w_kr2", [D, 128])
    gq = din("gq", [128, 4])
    gkv = din("gkv", [128, 4])
    w_uqh = din("w_uqh", [16, 512, 256])
    w_ukv = din("w_ukv", [512, 4096])
    ropeCS = din("ropeCS", [2, 64, T])
    ident_d = din("ident", [128, 128])
    tabI = din("tabI", [2, 5, 128, 1024])
    tabB = din("tabB", [2, 16, 128, 1024])
    tabM = din("tabM", [2, 16, 1024])
    w_na = din("w_na", [1024, D])
    w_mla = din("w_mla", [D, D])
    w_out = din("w_out", [D, D])
    PEER_ON = stop_after in (None, "G0", "G1")
    if PEER_ON:
        w_qT = din("w_qT", [16, 128, D])
        subkT = din("subkT", [16, 128, 128])
        puT = din("puT", [128, 128, D])
        pv2 = din("pv2", [128, 128, D])
        peer_c = din("peer_c", [128, 161])
    out_d = nc.dram_tensor("out", [S, D], F32, kind="ExternalOutput").ap()

    s_naq = dscr("s_naq", [8, 128, S], BF16)
    s_nak = dscr("s_nak", [8, 128, T], BF16)
    s_nav = dscr("s_nav", [T, 1024], BF16)
    s_ga = dscr("s_ga", [16, 128, S], BF16)
    s_gb = dscr("s_gb", [16, 128, S], BF16)
    s_omla = dscr("s_omla", [16, 128, S], BF16)
    s_ona = dscr("s_ona", [8, 128, S], BF16)
    s_h2 = dscr("s_h2", [S, D], F32)
    s_merged = dscr("s_merged", [16, 128, S], BF16)
    s_hn2T = dscr("s_hn2T", [8, 128, 16, 256], BF16)
    s_slot = dscr("s_slot", [16, 128, 384], F32)
    s_uT = dscr("s_uT", [128, 128, D], BF16)
    s_v2 = dscr("s_v2", [128, 128, D], BF16)
    d_cqn = dscr("d_cqn", [128, 4, S], BF16) if "d_cqn" in debug else None
    d_ckvn = dscr("d_ckvn", [128, 4, T], BF16) if "d_ckvn" in debug else None
    d_krope = dscr("d_krope", [128, T], BF16) if "d_krope" in debug else None
    d_merged = dscr("d_merged", [128, 16, S], BF16) if "d_merged" in debug else None
    R_scr = {k: Res() for k in ("naq", "nak", "nav", "ga", "gb", "omla", "ona", "h2", "dbg", "merged", "hn2T", "slot", "uT", "v2")}

    with ExitStack() as top:
        P = Prog(nc, top)
        sb = lambda st, name, shape, dt: _sb(nc, st, name, shape, dt)
        ps = lambda st, name, shape, dt: _ps(nc, st, name, shape, dt)

        ident_f = sb(top, "ident_f", [128, 128], F32)
        ident = sb(top, "identb", [128, 128], BF16)
        ones_bf = sb(top, "ones_bf", [128, 128], BF16)
        ones_f = sb(top, "ones_f", [128, 128], F32)
        R_const = Res()
        P.dma("sp", ident_f[:], ident_d, [], [R_const], "c0")
        P.op("dve", lambda e: e.tensor_copy(out=ident[:], in_=ident_f[:]), [R_const], [R_const])
        P.op("dve", lambda e: e.memset(ones_bf[:], 1.0), [], [R_const])
        P.op("dve", lambda e: e.memset(ones_f[:], 1.0), [], [R_const])

        def done():
            P.flush()

        pcb = {}

        def pc_alloc(st, tag):
            pcb["stg"] = [sb(st, f"pc_stg{tag}{i}", [128, D], F32) for i in range(2)]
            pcb["cst"] = [sb(st, f"pc_cst{tag}{i}", [128, D], BF16) for i in range(2)]
            pcb["Rs"] = [Res(), Res()]
            pcb["Rc"] = [Res(), Res()]
            pcb["fresh"] = True

        def _pc_gen():
            steps = [(src, dst, key, i2) for i2 in range(128) for src, dst, key in ((puT, s_uT, "uT"), (pv2, s_v2, "v2"))]

            def load(k):
                src, dst, key, i2 = steps[k]
                s = k % 2
                P.dma("pool", pcb["stg"][s][:], src[i2], [], [pcb["Rs"][s]], f"pcl{s}")

            for k in range(len(steps)):
                src, dst, key, i2 = steps[k]
                s = k % 2
                if k == 0 or pcb.get("fresh"):
                    pcb["fresh"] = False
                    load(k)
                stg, cst, R_pcs, R_pcc = pcb["stg"], pcb["cst"], pcb["Rs"], pcb["Rc"]
                if k + 1 < len(steps):
                    load(k + 1)
                P.op("pool", lambda e, s=s, stg=stg, cst=cst: e.tensor_copy(out=cst[s][:], in_=stg[s][:]),
                     [R_pcs[s]], [R_pcc[s]])
                P.dma("pool", dst[i2], cst[s][:], [R_pcc[s]], [R_scr[key]], f"pcs{s}")
                yield

        pc_it = _pc_gen() if PEER_ON else iter(())

        def pc_advance(n):
            for _ in range(n):
                if next(pc_it, "end") == "end":
                    break

        with ExitStack() as stAB:
            cqnT = sb(stAB, "cqnT", [128, 4, S], BF16)
            ckvnT = sb(stAB, "ckvnT", [128, 4, T], BF16)
            kropeT = sb(stAB, "kropeT", [128, T], BF16)
            ropeC = sb(stAB, "ropeC", [64, T], F32)
            ropeS = sb(stAB, "ropeS", [64, T], F32)
            R_cqn, R_ckvn, R_krope, R_rope = Res(), Res(), Res(), Res()
            P.dma("sp", ropeC[:], ropeCS[0], [], [R_rope], "c1")
            P.dma("sp", ropeS[:], ropeCS[1], [], [R_rope], "c1")
            P.op("dve", lambda e: e.memset(kropeT[:], 0.0), [], [R_krope])

            with ExitStack() as stB:
                hnT = sb(stB, "hnT", [128, 16, T], BF16)
                R_hnT = Res()
                with ExitStack() as st:
                    xt = [sb(st, f"xt{i}", [128, D], F32) for i in range(2)]
                    hn = [sb(st, f"hn{i}", [128, D], BF16) for i in range(2)]
                    junk = sb(st, "junkA", [128, D], BF16)
                    g1bc = sb(st, "g1bc", [128, D], F32)
                    ssq = [sb(st, f"ssq{i}", [128, 1], F32) for i in range(2)]
                    rt = [sb(st, f"rt{i}", [128, 1], F32) for i in range(2)]
                    rstd = [sb(st, f"rstd{i}", [128, 1], F32) for i in range(2)]
                    pT = [ps(st, f"pT{i}", [128, 16, 128], BF16) for i in range(2)]
                    R_xt, R_hn, R_ss, R_rt, R_rstd, R_pT = ([Res(), Res()] for _ in range(6))
                    R_junk, R_g = Res(), Res()
                    P.dma("sp", g1bc[:], g1.partition_broadcast(128), [], [R_g], "c3")
                    pend_a = []

                    def flush_copy_a():
                        s, rows, col0 = pend_a.pop(0)
                        P.op("act", lambda e: e.copy(out=hnT[:, :, col0:col0 + rows], in_=pT[s][:, :, :rows]),
                             [R_pT[s]], [R_hnT])

                    for i in range(17):
                        s = i % 2
                        rows = 128 if i < 16 else NM
                        src = x[i * 128:(i + 1) * 128, :] if i < 16 else meta
                        col0 = i * 128
                        P.dma("sp", xt[s][:rows, :], src, [], [R_xt[s]], f"xt{s}")
                        P.op("act", lambda e, s=s, rows=rows: e.activation(
                            out=junk[:rows, :], in_=xt[s][:rows, :], func=AF.Square, accum_out=ssq[s][:rows, :]),
                            [R_xt[s]], [R_junk, R_ss[s]])
                        P.op("act", lambda e, s=s, rows=rows: e.activation(
                            out=rt[s][:rows, :], in_=ssq[s][:rows, :], func=AF.Sqrt, scale=1.0 / D, bias=EPS),
                            [R_ss[s]], [R_rt[s]])
                        if pend_a:
                            flush_copy_a()
                        P.op("dve", lambda e, s=s, rows=rows: e.reciprocal(out=rstd[s][:rows, :], in_=rt[s][:rows, :]),
                             [R_rt[s]], [R_rstd[s]])
                        P.op("dve", lambda e, s=s, rows=rows: e.scalar_tensor_tensor(
                            out=hn[s][:rows, :], in0=xt[s][:rows, :], scalar=rstd[s][:rows, 0:1], in1=g1bc[:rows, :],
                            op0=ALU.mult, op1=ALU.mult), [R_xt[s], R_rstd[s], R_g], [R_hn[s]])
                        P.op("pe", [lambda e, s=s, rows=rows, dc=dc: e.transpose(
                            out=pT[s][:, dc, :rows], in_=hn[s][:rows, dc * 128:(dc + 1) * 128],
                            identity=ident[:rows, :rows]) for dc in range(16)], [R_hn[s], R_const], [R_pT[s]])
                        pend_a.append((s, rows, col0))
                    while pend_a:
                        flush_copy_a()
                    done()

                if stop_after == "A":
                    return nc
                with ExitStack() as st:
                    NW = 128
                    wst = [sb(st, f"wst{i}", [128, 16, NW], F32) for i in range(2)]
                    wbf = [sb(st, f"wbf{i}", [128, 16, NW], BF16) for i in range(2)]
                    R_wst, R_wbf = [Res(), Res()], [Res(), Res()]
                    acc = [ps(st, f"acc{i}", [128, 512], F32) for i in range(4)]
                    R_acc = [Res() for _ in range(4)]
                    outt = [sb(st, f"outt{i}", [128, T], BF16) for i in range(2)]
                    R_outt = [Res() for _ in range(2)]
                    cT = sb(st, "cT", [128, 4, T], F32)
                    R_cT = Res()
                    sqt = sb(st, "sqt", [128, 4, 512], F32)
                    rtt = sb(st, "rtt", [128, 512], F32)
                    rstdbc = sb(st, "rstdbc", [128, 512], F32)
                    R_sqt, R_rtt, R_rbc = Res(), Res(), Res()
                    gcol = sb(st, "gcol", [128, 8], F32)
                    R_gcol = Res()
                    t1 = sb(st, "t1", [64, 512], F32)
                    t2 = sb(st, "t2", [64, 512], F32)
                    R_t1, R_t2 = Res(), Res()
                    vout = [sb(st, f"vout{i}", [128, 4, 128], BF16) for i in range(2)]
                    R_vout = [Res() for _ in range(2)]
                    pTv = [ps(st, f"pTv{i}", [128, 512], BF16) for i in range(2)]
                    R_pTv = [Res(), Res()]
                    blocks_nav = TB
                    P.dma("sp", gcol[:, 0:4], gq, [], [R_gcol], "c4")
                    P.dma("sp", gcol[:, 4:8], gkv, [], [R_gcol], "c4")

                    groups = []
                    for j in range(8):
                        groups.append(("naq", w_in[:, j * 128:(j + 1) * 128], 128, QB, j))
                    for j in range(8):
                        groups.append(("nak", w_in[:, 1024 + j * 128:1024 + (j + 1) * 128], 128, TB, j))
                    for j in range(4):
                        groups.append(("cq", w_in[:, 3072 + j * 128:3072 + (j + 1) * 128], 128, QB, j))
                    for j in range(16):
                        groups.append(("ga", w_in[:, 4160 + j * 128:4160 + (j + 1) * 128], 128, QB, j))
                    for j in range(4):
                        groups.append(("ckv", w_in[:, 3584 + j * 128:3584 + (j + 1) * 128], 128, TB, j))
                    for j in range(16):
                        groups.append(("gb", w_in[:, 6208 + j * 128:6208 + (j + 1) * 128], 128, QB, j))
                    groups.append(("kr", w_kr2, 128, TB, 0))
                    for j in range(8):
                        groups.append(("nav", w_in[:, 2048 + j * 128:2048 + (j + 1) * 128], 128, None, j))
                    norm_it = [iter(())]

                    def load_group(gi):
                        kind, wap, n, blocks, j = groups[gi]
                        s = gi % 2
                        P.dma("sp", wst[s][:, :, :n], wap.rearrange("(c p) f -> p c f", p=128), [], [R_wst[s]], f"wst{s}")
                        P.op("pool", lambda e, s=s, n=n: e.tensor_copy(out=wbf[s][:, :, :n], in_=wst[s][:, :, :n]),
                             [R_wst[s]], [R_wbf[s]])

                    cnt = {"acc": 0, "outt": 0, "vout": 0}

                    def next_acc():
                        a = cnt["acc"] % 4
                        cnt["acc"] += 1
                        return a

                    def proj_fm(s, f0, m, c0, n, a):
                        P.op("pe", [lambda e, dc=dc: e.matmul(
                            out=acc[a][:m, :n], lhsT=wbf[s][:, dc, f0:f0 + m], rhs=hnT[:, dc, c0:c0 + n],
                            start=(dc == 0), stop=(dc == 15)) for dc in range(16)],
                            [R_wbf[s], R_hnT], [R_acc[a]])

                    def c_norm(ncols, blocks, gofs, dstT, R_dst):
                        for (c0, n) in blocks:
                            yield
                            P.op("act", lambda e, c0=c0, n=n: e.activation(
                                out=sqt[:, :, :n], in_=cT[:, :, c0:c0 + n], func=AF.Square), [R_cT], [R_sqt])
                            a = next_acc()
                            P.op("pe", [lambda e, rc=rc, n=n, a=a: e.matmul(
                                out=acc[a][:, :n], lhsT=ones_f[:], rhs=sqt[:, rc, :n],
                                start=(rc == 0), stop=(rc == 3)) for rc in range(4)], [R_sqt, R_const], [R_acc[a]])
                            P.op("act", lambda e, n=n, a=a: e.activation(
                                out=rtt[:, :n], in_=acc[a][:, :n], func=AF.Sqrt, scale=1.0 / 512, bias=EPS),
                                [R_acc[a]], [R_rtt])
                            P.op("dve", lambda e, n=n: e.reciprocal(out=rstdbc[:, :n], in_=rtt[:, :n]),
                                 [R_rtt], [R_rbc])
                            for rc in range(4):
                                P.op("dve", lambda e, rc=rc, c0=c0, n=n: e.scalar_tensor_tensor(
                                    out=dstT[:, rc, c0:c0 + n], in0=cT[:, rc, c0:c0 + n],
                                    scalar=gcol[:, gofs + rc:gofs + rc + 1],
                                    in1=rstdbc[:, :n], op0=ALU.mult, op1=ALU.mult), [R_cT, R_gcol, R_rbc], [R_dst])

                    load_group(0)
                    for gi, (kind, wap, n, blocks, j) in enumerate(groups):
                        if gi + 1 < len(groups):
                            load_group(gi + 1)
                        s = gi % 2
                        if kind in ("ga", "gb", "kr"):
                            next(norm_it[0], None)
                        if kind in ("naq", "nak", "ga", "gb"):
                            func = AF.Sigmoid if kind in ("ga", "gb") else AF.Copy
                            dst = {"naq": s_naq, "nak": s_nak, "ga": s_ga, "gb": s_gb}[kind]
                            ntok = S if blocks is QB else T
                            o = cnt["outt"] % 2
                            cnt["outt"] += 1
                            for (c0, nn) in blocks:
                                a = next_acc()
                                proj_fm(s, 0, 128, c0, nn, a)
                                P.op("act", lambda e, o=o, a=a, c0=c0, nn=nn, func=func: e.activation(
                                    out=outt[o][:, c0:c0 + nn], in_=acc[a][:, :nn], func=func),
                                    [R_acc[a]], [R_outt[o]])
                            P.dma("act", dst[j], outt[o][:, :ntok], [R_outt[o]], [R_scr[kind]], f"outt{o}")
                        elif kind in ("cq", "ckv"):
                            rc = j
                            for (c0, nn) in blocks:
                                a = next_acc()
                                proj_fm(s, 0, 128, c0, nn, a)
                                P.op("act", lambda e, a=a, c0=c0, nn=nn, rc=rc: e.copy(
                                    out=cT[:, rc, c0:c0 + nn], in_=acc[a][:, :nn]), [R_acc[a]], [R_cT])
                            if j == 3:
                                for _ in norm_it[0]:
                                    pass
                                if kind == "cq":
                                    norm_it[0] = c_norm(S, QB, 0, cqnT, R_cqn)
                                else:
                                    norm_it[0] = c_norm(T, TB, 4, ckvnT, R_ckvn)
                        elif kind == "kr":
                            for (c0, nn) in blocks:
                                a = next_acc()
                                b = next_acc()
                                proj_fm(s, 0, 64, c0, nn, a)
                                proj_fm(s, 64, 64, c0, nn, b)
                                P.op("dve", lambda e, a=a, c0=c0, nn=nn: e.tensor_tensor(
                                    out=t1[:, :nn], in0=acc[a][:64, :nn], in1=ropeC[:, c0:c0 + nn], op=ALU.mult),
                                    [R_acc[a], R_rope], [R_t1])
                                P.op("dve", lambda e, b=b, c0=c0, nn=nn: e.tensor_tensor(
                                    out=t2[:, :nn], in0=acc[b][:64, :nn], in1=ropeS[:, c0:c0 + nn], op=ALU.mult),
                                    [R_acc[b], R_rope], [R_t2])
                                P.op("dve", lambda e, c0=c0, nn=nn: e.tensor_tensor(
                                    out=kropeT[0:64, c0:c0 + nn], in0=t1[:, :nn], in1=t2[:, :nn], op=ALU.add),
                                    [R_t1, R_t2], [R_krope])
                        elif kind == "nav":
                            o = cnt["outt"] % 2
                            cnt["outt"] += 1
                            for (c0, nn) in blocks_nav:
                                a = next_acc()
                                proj_fm(s, 0, 128, c0, nn, a)
                                P.op("act", lambda e, o=o, a=a, c0=c0, nn=nn: e.copy(
                                    out=outt[o][:, c0:c0 + nn], in_=acc[a][:, :nn]), [R_acc[a]], [R_outt[o]])
                            for g4 in range(5):
                                tiles = list(range(4 * g4, min(4 * g4 + 4, 17)))
                                v_ = cnt["vout"] % 2
                                cnt["vout"] += 1
                                p_ = cnt["vout"] % 2
                                fns = []
                                for jj, ti in enumerate(tiles):
                                    rows = 128 if ti < 16 else NM
                                    fns.append(lambda e, jj=jj, ti=ti, rows=rows, o=o, p_=p_: e.transpose(
                                        out=pTv[p_][:rows, jj * 128:(jj + 1) * 128], in_=outt[o][:, ti * 128:ti * 128 + rows],
                                        identity=ident[:]))
                                P.op("pe", fns, [R_outt[o], R_const], [R_pTv[p_]])
                                if g4 < 4:
                                    P.op("act", lambda e, v_=v_, p_=p_: e.copy(
                                        out=vout[v_][:].rearrange("p t c -> p (t c)"), in_=pTv[p_][:, :]),
                                        [R_pTv[p_]], [R_vout[v_]])
                                    P.dma("act", s_nav[g4 * 512:(g4 + 1) * 512, j * 128:(j + 1) * 128].rearrange(
                                        "(t p) c -> p t c", p=128), vout[v_][:], [R_vout[v_]], [R_scr["nav"]], f"vout{v_}")
                                else:
                                    P.op("act", lambda e, v_=v_, p_=p_: e.copy(out=vout[v_][:NM, 0, :], in_=pTv[p_][:NM, 0:128]),
                                         [R_pTv[p_]], [R_vout[v_]])
                                    P.dma("act", s_nav[S:T, j * 128:(j + 1) * 128], vout[v_][:NM, 0, :],
                                          [R_vout[v_]], [R_scr["nav"]], f"vout{v_}")
                    for _ in norm_it[0]:
                        pass
                    if d_cqn is not None:
                        P.dma("sp", d_cqn, cqnT[:], [R_cqn], [R_scr["dbg"]], "dbg")
                    if d_ckvn is not None:
                        P.dma("sp", d_ckvn, ckvnT[:], [R_ckvn], [R_scr["dbg"]], "dbg")
                    if d_krope is not None:
                        P.dma("sp", d_krope, kropeT[:], [R_krope], [R_scr["dbg"]], "dbg")
                    done()
            if stop_after == "B":
                return nc
            with ExitStack() as st:
                pc_alloc(st, "C")
                wst = [sb(st, f"cwst{i}", [128, 8, 256], F32) for i in range(2)]
                wbf = [sb(st, f"cwbf{i}", [128, 8, 256], BF16) for i in range(2)]
                qnT = [sb(st, f"qnT{i}", [128, S], BF16) for i in range(2)]
                qrT = [sb(st, f"qrT{i}", [128, S], BF16) for i in range(2)]
                knT = [sb(st, f"knT{i}", [128, T], BF16) for i in range(2)]
                vtm = [sb(st, f"vtm{i}", [128, 17, 128], BF16) for i in range(2)]
                oT = [sb(st, f"oT{i}", [128, S], BF16) for i in range(2)]
                ptl = [sb(st, f"ptl{i}", [128, 512], BF16) for i in range(3)]
                rct = sb(st, "crct", [128, 512], F32)
                t1 = sb(st, "ct1", [64, 512], F32)
                t2 = sb(st, "ct2", [64, 512], F32)
                rxa = sb(st, "crxa", [64, 512], F32)
                rxb = sb(st, "crxb", [64, 512], F32)
                R_rxa, R_rxb = Res(), Res()
                pa = [ps(st, f"cpa{i}", [128, 512], F32) for i in range(2)]
                pS = [ps(st, f"cpS{i}", [128, 512], F32) for i in range(2)]
                pO = [ps(st, f"cpO{i}", [128, 512], F32) for i in range(2)]
                pSum = [ps(st, f"cpSum{i}", [128, 512], F32) for i in range(2)]
                R_wst, R_wbf, R_qn, R_qr, R_kn, R_v, R_oT, R_pa, R_pS, R_pO = ([Res(), Res()] for _ in range(10))
                R_pt = [Res() for _ in range(3)]
                R_rct, R_t1, R_t2 = Res(), Res(), Res()
                for i in range(2):
                    P.op("dve", lambda e, i=i: e.memset(qrT[i][:], 0.0), [], [R_qr[i]])
                cc = {"pa": 0}

                def next_pa():
                    a = cc["pa"] % 2
                    cc["pa"] += 1
                    return a

                def load_w(h):
                    s = h % 2
                    P.dma("sp", wst[s][:, 0:4, :], w_uqh[h].rearrange("(c p) f -> p c f", p=128), [], [R_wst[s]], f"cwst{s}")
                    P.dma("sp", wst[s][:, 4:8, :], w_ukv[:, h * 256:(h + 1) * 256].rearrange("(c p) f -> p c f", p=128),
                          [], [R_wst[s]], f"cwst{s}")
                    P.op("pool", lambda e, s=s: e.tensor_copy(out=wbf[s][:], in_=wst[s][:]), [R_wst[s]], [R_wbf[s]])

                def project(h):
                    s = h % 2

                    def qn_blk(c0, n):
                        a = next_pa()
                        P.op("pe", [lambda e, rc=rc: e.matmul(
                            out=pa[a][:, :n], lhsT=wbf[s][:, rc, 0:128], rhs=cqnT[:, rc, c0:c0 + n],
                            start=(rc == 0), stop=(rc == 3)) for rc in range(4)], [R_wbf[s], R_cqn], [R_pa[a]])
                        P.op("act", lambda e: e.copy(out=qnT[s][:, c0:c0 + n], in_=pa[a][:, :n]), [R_pa[a]], [R_qn[s]])

                    def rope_blk(c0, n):
                        a = next_pa()
                        P.op("pe", [lambda e, rc=rc: e.matmul(
                            out=pa[a][:64, :n], lhsT=wbf[s][:, rc, 128:192], rhs=cqnT[:, rc, c0:c0 + n],
                            start=(rc == 0), stop=(rc == 3)) for rc in range(4)], [R_wbf[s], R_cqn], [R_pa[a]])
                        P.op("act", lambda e: e.copy(out=rxa[:, :n], in_=pa[a][:64, :n]), [R_pa[a]], [R_rxa])
                        P.op("dve", lambda e: e.tensor_tensor(
                            out=t1[:, :n], in0=rxa[:, :n], in1=ropeC[:, c0:c0 + n], op=ALU.mult),
                            [R_rxa, R_rope], [R_t1])
                        b = next_pa()
                        P.op("pe", [lambda e, rc=rc: e.matmul(
                            out=pa[b][:64, :n], lhsT=wbf[s][:, rc, 192:256], rhs=cqnT[:, rc, c0:c0 + n],
                            start=(rc == 0), stop=(rc == 3)) for rc in range(4)], [R_wbf[s], R_cqn], [R_pa[b]])
                        P.op("act", lambda e: e.copy(out=rxb[:, :n], in_=pa[b][:64, :n]), [R_pa[b]], [R_rxb])
                        P.op("dve", lambda e: e.tensor_tensor(
                            out=t2[:, :n], in0=rxb[:, :n], in1=ropeS[:, c0:c0 + n], op=ALU.mult),
                            [R_rxb, R_rope], [R_t2])
                        P.op("dve", lambda e: e.tensor_tensor(
                            out=qrT[s][0:64, c0:c0 + n], in0=t1[:, :n], in1=t2[:, :n], op=ALU.add),
                            [R_t1, R_t2], [R_qr[s]])

                    def kn_blk(c0, n):
                        a = next_pa()
                        P.op("pe", [lambda e, rc=rc: e.matmul(
                            out=pa[a][:, :n], lhsT=wbf[s][:, 4 + rc, 0:128], rhs=ckvnT[:, rc, c0:c0 + n],
                            start=(rc == 0), stop=(rc == 3)) for rc in range(4)], [R_wbf[s], R_ckvn], [R_pa[a]])
                        P.op("act", lambda e: e.copy(out=knT[s][:, c0:c0 + n], in_=pa[a][:, :n]), [R_pa[a]], [R_kn[s]])

                    def v_grp(g):
                        tiles = list(range(4 * g, min(4 * g + 4, 17)))
                        a = next_pa()
                        fns = []
                        for jj, ti in enumerate(tiles):
                            rows = 128 if ti < 16 else NM
                            for rc in range(4):
                                fns.append(lambda e, rc=rc, jj=jj, ti=ti, rows=rows: e.matmul(
                                    out=pa[a][:rows, jj * 128:(jj + 1) * 128], lhsT=ckvnT[:, rc, ti * 128:ti * 128 + rows],
                                    rhs=wbf[s][:, 4 + rc, 128:256], start=(rc == 0), stop=(rc == 3)))
                        P.op("pe", fns, [R_wbf[s], R_ckvn], [R_pa[a]])
                        if g < 4:
                            P.op("act", lambda e: e.copy(
                                out=vtm[s][:, 4 * g:4 * g + 4, :], in_=pa[a][:, :].rearrange("p (j c) -> p j c", c=128)),
                                [R_pa[a]], [R_v[s]])
                        else:
                            P.op("act", lambda e: e.copy(out=vtm[s][:NM, 16, :], in_=pa[a][:NM, 0:128]),
                                 [R_pa[a]], [R_v[s]])

                    for bi, (c0, n) in enumerate(QB):
                        rope_blk(c0, n)
                        qn_blk(c0, n)
                        kn_blk(c0, n)
                        v_grp(bi)
                    kn_blk(*TB[4])
                    v_grp(4)

                def attention(h):
                    s = h % 2
                    items = [(qb, kt) for qb in range(4) for kt in range(17)]

                    def s_step(idx):
                        qb, kt = items[idx]
                        c0 = qb * 512
                        kr = 128 if kt < 16 else NM
                        k0 = kt * 128
                        p_ = idx % 2
                        P.op("pe", [
                            lambda e: e.matmul(out=pS[p_][:kr, :], lhsT=knT[s][:, k0:k0 + kr], rhs=qnT[s][:, c0:c0 + 512],
                                               start=True, stop=False),
                            lambda e: e.matmul(out=pS[p_][:kr, :], lhsT=kropeT[:, k0:k0 + kr], rhs=qrT[s][:, c0:c0 + 512],
                                               start=False, stop=True)],
                            [R_kn[s], R_qn[s], R_krope, R_qr[s]], [R_pS[p_]])
                        P.op("act", lambda e: e.activation(out=ptl[idx % 3][:kr, :], in_=pS[p_][:kr, :], func=AF.Exp,
                                                           scale=MLA_SCALE), [R_pS[p_]], [R_pt[idx % 3]])

                    def pv_step(idx):
                        qb, kt = items[idx]
                        c0 = qb * 512
                        kr = 128 if kt < 16 else NM
                        ob = qb % 2
                        P.op("pe", [
                            lambda e: e.matmul(out=pO[ob][:, :], lhsT=vtm[s][:kr, kt, :], rhs=ptl[idx % 3][:kr, :],
                                               start=(kt == 0), stop=(kt == 16)),
                            lambda e: e.matmul(out=pSum[ob][:, :], lhsT=ones_bf[:kr, :], rhs=ptl[idx % 3][:kr, :],
                                               start=(kt == 0), stop=(kt == 16))],
                            [R_v[s], R_pt[idx % 3], R_const], [R_pO[ob]])
                        if kt == 16:
                            P.op("dve", lambda e: e.reciprocal(out=rct[:], in_=pSum[ob][:, :]), [R_pO[ob]], [R_rct])
                            P.op("dve", lambda e: e.tensor_tensor(out=oT[s][:, c0:c0 + 512], in0=pO[ob][:, :], in1=rct[:],
                                                                  op=ALU.mult), [R_pO[ob], R_rct], [R_oT[s]])

                    for idx in range(len(items) + 1):
                        if idx < len(items):
                            s_step(idx)
                        if idx >= 1:
                            pv_step(idx - 1)
                    P.dma("sp", s_omla[h], oT[s][:], [R_oT[s]], [R_scr["omla"]], f"oT{s}")

                NH = 16
                load_w(0)
                load_w(1)
                project(0)
                for h in range(NH):
                    if h + 1 < NH:
                        project(h + 1)
                    if h + 2 < NH:
                        load_w(h + 2)
                    pc_advance(10)
                    attention(h)
                done()
        if stop_after == "C":
            return nc

        with ExitStack() as st:
            pc_alloc(st, "D")
            qh = sb(st, "qh", [128, 4, S], BF16)
            kh = sb(st, "kh", [128, 4, T], BF16)
            vh = sb(st, "vh", [128, 17, 512], BF16)
            tI = sb(st, "tI", [128, 5, 1024], F32)
            tBd2 = [sb(st, f"tBd{i}", [128, 4, 1024], F32) for i in range(2)]
            R_tB2 = [Res(), Res()]
            bslot = {0: 0, 1: 1, 14: 0, 15: 1}
            tM = sb(st, "tM", [NM, 1024], F32)
            sbias = [sb(st, f"sbias{i}", [128, 1024], F32) for i in range(2)]
            ptd = [sb(st, f"ptd{i}", [128, 6, 1024], BF16) for i in range(2)]
            onaT = sb(st, "onaT", [128, 4, S], BF16)
            rct = sb(st, "drct", [128, 512], F32)
            pS = [ps(st, f"dpS{i}", [128, 1024], F32) for i in range(2)]
            pO = [ps(st, f"dpO{i}", [128, 512], F32) for i in range(2)]
            pSum = [ps(st, f"dpSum{i}", [128, 512], F32) for i in range(2)]
            R_q, R_k, R_v, R_tI, R_tB, R_tM, R_ona, R_rct = (Res() for _ in range(8))
            R_sb, R_pt, R_pS, R_pO = ([Res(), Res()] for _ in range(4))
            cS = {"n": 0}
            bmap = {0: 0, 1: 1, 14: 2, 15: 3}

            for hp in range(2):
                def load_btab(i, q="sp"):
                    b = bmap[i]
                    P.dma(q, tBd2[bslot[i]][:], tabB[hp, 4 * b:4 * b + 4].rearrange("d k f -> k d f"), [],
                          [R_tB2[bslot[i]]], f"dtB{bslot[i]}")

                P.dma("sp", qh[:], s_naq[4 * hp:4 * hp + 4].rearrange("c p t -> p c t"), [R_scr["naq"]], [R_q], "dq")
                P.dma("act", kh[:], s_nak[4 * hp:4 * hp + 4].rearrange("c p t -> p c t"), [R_scr["nak"]], [R_k], "dk")
                load_btab(0, "pool")
                P.dma("sp", tM[:], tabM[hp], [], [R_tM], "dtM")
                P.dma("act", vh[:, 0:16, :], s_nav[0:S, hp * 512:(hp + 1) * 512].rearrange("(t p) c -> p t c", p=128),
                      [R_scr["nav"]], [R_v], "dv")
                P.dma("act", vh[:NM, 16, :], s_nav[S:T, hp * 512:(hp + 1) * 512], [R_scr["nav"]], [R_v], "dv")
                load_btab(1, "pool")
                P.dma("sp", tI[:], tabI[hp].rearrange("d k f -> k d f"), [], [R_tI], "dtI")

                def tile_plan(i):
                    if i in bmap:
                        k0 = 0 if i < 2 else 12
                        plan = [(k0 + j, 128, tBd2[bslot[i]], j, R_tB2[bslot[i]]) for j in range(4)]
                    else:
                        plan = [(i - 2 + j, 128, tI, j, R_tI) for j in range(5)]
                    plan.append((16, NM, tM, None, R_tM))
                    return plan

                def s_phase(i):
                    pb = i % 2
                    for j, (kt, kr, tab, tj, R_tab) in enumerate(tile_plan(i)):
                        p_ = cS["n"] % 2
                        cS["n"] += 1
                        P.op("pe", [lambda e, e_=e_, pp=pp, kt=kt, kr=kr, p_=p_, i=i: e.matmul(
                            out=pS[p_][:kr, e_ * 512 + pp * 128:e_ * 512 + (pp + 1) * 128],
                            lhsT=kh[64 * e_:64 * e_ + 64, pp, kt * 128:kt * 128 + kr],
                            rhs=qh[64 * e_:64 * e_ + 64, pp, i * 128:(i + 1) * 128], start=True, stop=True)
                            for pp in range(4) for e_ in range(2)], [R_k, R_q], [R_pS[p_]])
                        tab_ap = tab[:kr, tj, :] if tj is not None else tab[:kr, :]
                        P.op("dve", lambda e, p_=p_, kr=kr, tab_ap=tab_ap: e.scalar_tensor_tensor(
                            out=sbias[p_][:kr, :], in0=pS[p_][:kr, :], scalar=NA_SCALE, in1=tab_ap,
                            op0=ALU.mult, op1=ALU.add), [R_pS[p_], R_tab], [R_sb[p_]])
                        P.op("act", lambda e, p_=p_, kr=kr, j=j, pb=pb: e.activation(
                            out=ptd[pb][:kr, j, :], in_=sbias[p_][:kr, :], func=AF.Exp), [R_sb[p_]], [R_pt[pb]])

                def pv_phase(i):
                    pb = i % 2
                    ob = i % 2
                    plan = tile_plan(i)
                    nj = len(plan)
                    fns = []
                    for pp in range(4):
                        for j, (kt, kr, tab, tj, R_tab) in enumerate(plan):
                            for e_ in range(2):
                                hh = e_ * 4 + pp
                                vc = (2 * pp + e_) * 64
                                fns.append(lambda e, e_=e_, pp=pp, hh=hh, vc=vc, j=j, kt=kt, kr=kr: e.matmul(
                                    out=pO[ob][64 * e_:64 * e_ + 64, pp * 128:(pp + 1) * 128],
                                    lhsT=vh[:kr, kt, vc:vc + 64], rhs=ptd[pb][:kr, j, hh * 128:(hh + 1) * 128],
                                    start=(j == 0), stop=(j == nj - 1)))
                    for j, (kt, kr, tab, tj, R_tab) in enumerate(plan):
                        for e_ in range(2):
                            fns.append(lambda e, e_=e_, j=j, kr=kr: e.matmul(
                                out=pSum[ob][64 * e_:64 * e_ + 64, :], lhsT=ones_bf[:kr, 0:64],
                                rhs=ptd[pb][:kr, j, e_ * 512:(e_ + 1) * 512], start=(j == 0), stop=(j == nj - 1)))
                    P.op("pe", fns, [R_v, R_pt[pb], R_const], [R_pO[ob]])

                def pv_epi(i):
                    ob = i % 2
                    P.op("dve", lambda e: e.reciprocal(out=rct[:], in_=pSum[ob][:, :]), [R_pO[ob]], [R_rct])
                    P.op("dve", lambda e: e.tensor_tensor(
                        out=onaT[:, :, i * 128:(i + 1) * 128], in0=pO[ob][:, :].rearrange("p (c q) -> p c q", q=128),
                        in1=rct[:, :].rearrange("p (c q) -> p c q", q=128), op=ALU.mult), [R_pO[ob], R_rct], [R_ona])

                for i in range(18):
                    if i < 16:
                        s_phase(i)
                    if i == 1:
                        load_btab(14)
                        load_btab(15)
                    pc_advance(2)
                    if 2 <= i:
                        pv_epi(i - 2)
                    if 1 <= i <= 16:
                        pv_phase(i - 1)
                P.dma("sp", s_ona[4 * hp:4 * hp + 4].rearrange("c p t -> p c t"), onaT[:], [R_ona], [R_scr["ona"]], "dona")
            done()
        if stop_after == "D":
            return nc

        with ExitStack() as st:
            pc_alloc(st, "E")
            onaT = sb(st, "e_onaT", [128, 8, S], BF16)
            omlaT = sb(st, "e_omlaT", [128, 16, S], BF16)
            wst = [sb(st, f"ewst{i}", [128, 24, 128], F32) for i in range(2)]
            wbf = [sb(st, f"ewbf{i}", [128, 24, 128], BF16) for i in range(2)]
            gat = [sb(st, f"egat{i}", [128, 2, S], BF16) for i in range(2)]
            mrg = [sb(st, f"emrg{i}", [128, S], BF16) for i in range(2)]
            t1 = sb(st, "et1", [128, 512], F32)
            t2 = sb(st, "et2", [128, 512], F32)
            pA = [ps(st, f"epA{i}", [128, 512], F32) for i in range(2)]
            pB = [ps(st, f"epB{i}", [128, 512], F32) for i in range(2)]
            R_t1, R_t2 = Res(), Res()
            R_wst, R_wbf, R_gat, R_mrg, R_pA, R_pB = ([Res(), Res()] for _ in range(6))
            R_ona2 = [Res() for _ in range(4)]
            R_omla2 = [Res() for _ in range(4)]

            def load_o(qb):
                c0 = qb * 512
                P.dma("sp", onaT[:, :, c0:c0 + 512], s_ona[:, :, c0:c0 + 512].rearrange("c p t -> p c t"),
                      [R_scr["ona"]], [R_ona2[qb]], f"eona{qb}")
                P.dma("act", omlaT[:, :, c0:c0 + 512], s_omla[:, :, c0:c0 + 512].rearrange("c p t -> p c t"),
                      [R_scr["omla"]], [R_omla2[qb]], f"eomla{qb}")

            def load_e(fo):
                s = fo % 2
                P.dma("sp", wst[s][:, 0:8, :], w_na[:, fo * 128:(fo + 1) * 128].rearrange("(c p) f -> p c f", p=128),
                      [], [R_wst[s]], f"ewst{s}")
                P.dma("sp", wst[s][:, 8:24, :], w_mla[:, fo * 128:(fo + 1) * 128].rearrange("(c p) f -> p c f", p=128),
                      [], [R_wst[s]], f"ewst{s}")
                P.dma("act", gat[s][:, 0, :], s_ga[fo], [R_scr["ga"]], [R_gat[s]], f"egat{s}")
                P.dma("act", gat[s][:, 1, :], s_gb[fo], [R_scr["gb"]], [R_gat[s]], f"egat{s}")
                P.op("act", lambda e, s=s: e.copy(out=wbf[s][:], in_=wst[s][:]), [R_wst[s]], [R_wbf[s]])

            load_e(0)
            load_o(0)
            for qb_ in range(1, 4):
                load_o(qb_)
            k = 0
            for fo in range(16):
                if fo + 1 < 16:
                    load_e(fo + 1)
                pc_advance(2)
                s = fo % 2
                for (c0, n) in QB:
                    p_ = k % 2
                    k += 1
                    P.op("pe", [lambda e, c=c, p_=p_, c0=c0, s=s: e.matmul(
                        out=pA[p_][:, :], lhsT=wbf[s][:, c, :], rhs=onaT[:, c, c0:c0 + 512],
                        start=(c == 0), stop=(c == 7)) for c in range(8)], [R_wbf[s], R_ona2[c0 // 512]], [R_pA[p_]])
                    P.op("pe", [lambda e, c=c, p_=p_, c0=c0, s=s: e.matmul(
                        out=pB[p_][:, :], lhsT=wbf[s][:, 8 + c, :], rhs=omlaT[:, c, c0:c0 + 512],
                        start=(c == 0), stop=(c == 15)) for c in range(16)], [R_wbf[s], R_omla2[c0 // 512]], [R_pB[p_]])
                    P.op("dve", lambda e, p_=p_, c0=c0, s=s: e.tensor_tensor(
                        out=t1[:], in0=pA[p_][:, :], in1=gat[s][:, 0, c0:c0 + 512], op=ALU.mult),
                        [R_pA[p_], R_gat[s]], [R_t1])
                    P.op("dve", lambda e, p_=p_, c0=c0, s=s: e.tensor_tensor(
                        out=t2[:], in0=pB[p_][:, :], in1=gat[s][:, 1, c0:c0 + 512], op=ALU.mult),
                        [R_pB[p_], R_gat[s]], [R_t2])
                    P.op("dve", lambda e, c0=c0, s=s: e.tensor_tensor(
                        out=mrg[s][:, c0:c0 + 512], in0=t1[:], in1=t2[:], op=ALU.add), [R_t1, R_t2], [R_mrg[s]])
                P.dma("sp", s_merged[fo], mrg[s][:], [R_mrg[s]], [R_scr["merged"]], f"emrg{s}")
            pc_advance(10 ** 6)
            done()
        if stop_after == "E":
            return nc

        with ExitStack() as st:
            mT = sb(st, "f_mT", [128, 16, S], BF16)
            wo = sb(st, "f_wo", [128, 16, D], BF16)
            wst = [sb(st, f"fwst{i}", [128, 16, 128], F32) for i in range(2)]
            xt = [sb(st, f"fxt{i}", [128, D], F32) for i in range(2)]
            h2t = [sb(st, f"fh2{i}", [128, D], F32) for i in range(2)]
            pa = [ps(st, f"fpa{i}", [128, 512], F32) for i in range(8)]
            R_mT = Res()
            R_wo = [Res() for _ in range(4)]
            R_wst, R_xt, R_h2 = ([Res(), Res()] for _ in range(3))
            R_pa = [Res() for _ in range(8)]
            def load_wo(g):
                s = g % 2
                P.dma("sp" if s == 0 else "act", wst[s][:], w_out[:, g * 128:(g + 1) * 128].rearrange("(c p) f -> p c f", p=128),
                      [], [R_wst[s]], f"fwst{s}")
                P.op("dve", lambda e, s=s, g=g: e.tensor_copy(out=wo[:, :, g * 128:(g + 1) * 128], in_=wst[s][:]),
                     [R_wst[s]], [R_wo[g // 4]])

            load_wo(0)
            R_mTb = [Res() for _ in range(4)]
            for gg in range(4):
                P.dma("pool", mT[:, :, gg * 512:(gg + 1) * 512], s_merged[:, :, gg * 512:(gg + 1) * 512].rearrange("c p t -> p c t"),
                      [R_scr["merged"]], [R_mTb[gg]], f"fmT{gg}")
            for g in range(1, 4):
                load_wo(g)
            for i in range(16):
                s = i % 2
                P.dma("sp", xt[s][:], x[i * 128:(i + 1) * 128, :], [], [R_xt[s]], f"fxt{s}")
                for fb in range(4):
                    a = s * 4 + fb
                    if i == 0 and fb < 3:
                        for g in range(4 * fb + 4, 4 * fb + 8):
                            load_wo(g)
                    P.op("pe", [lambda e, c=c, a=a, i=i, fb=fb: e.matmul(
                        out=pa[a][:, :], lhsT=mT[:, c, i * 128:(i + 1) * 128], rhs=wo[:, c, fb * 512:(fb + 1) * 512],
                        start=(c == 0), stop=(c == 15)) for c in range(16)], [R_mTb[i // 4], R_wo[fb]], [R_pa[a]])
                    P.op("dve", lambda e, a=a, s=s, fb=fb: e.tensor_tensor(
                        out=h2t[s][:, fb * 512:(fb + 1) * 512], in0=pa[a][:, :], in1=xt[s][:, fb * 512:(fb + 1) * 512],
                        op=ALU.add), [R_pa[a], R_xt[s]], [R_h2[s]])
                P.dma("sp", s_h2[i * 128:(i + 1) * 128, :], h2t[s][:], [R_h2[s]], [R_scr["h2"]], f"fh2{s}")
            done()
        if stop_after == "F":
            return nc
        with ExitStack() as st:
            Wp = sb(st, "g_Wp", [128, 16, D], BF16)
            g2bc = sb(st, "g_g2bc", [128, D], F32)
            pcst = sb(st, "g_pcst", [128, 33], F32)
            R_Wp, R_gc = Res(), Res()
            P.dma("sp", g2bc[:], g2.partition_broadcast(128), [], [R_gc], "gc")
            P.dma("sp", pcst[:], peer_c[:, 0:33], [], [R_gc], "gc")
            iota16 = pcst[:, 0:16]
            thr17 = pcst[:, 16:33]
            with ExitStack() as st2:
                wqs = [sb(st2, f"g_wqs{i}", [128, D], F32) for i in range(2)]
                wqb = [sb(st2, f"g_wqb{i}", [128, D], BF16) for i in range(2)]
                sks = sb(st2, "g_sks", [128, 16, 128], F32)
                skb = sb(st2, "g_skb", [128, 16, 128], BF16)
                pw = [ps(st2, f"g_pw{i}", [128, 512], F32) for i in range(2)]
                R_wqs, R_wqb, R_pw = ([Res(), Res()] for _ in range(3))
                R_sk = Res()
                P.dma("sp", sks[:], subkT.rearrange("h c n -> c h n"), [], [R_sk], "gsk")
                P.op("dve", lambda e: e.tensor_copy(out=skb[:], in_=sks[:]), [R_sk], [R_sk])
                k = 0
                for hs in range(16):
                    s = hs % 2
                    P.dma("sp", wqs[s][:], w_qT[hs], [], [R_wqs[s]], f"gwq{s}")
                    P.op("dve", lambda e, s=s: e.tensor_copy(out=wqb[s][:], in_=wqs[s][:]), [R_wqs[s]], [R_wqb[s]])
                    for g in range(4):
                        p_ = k % 2
                        k += 1
                        P.op("pe", [lambda e, jj=jj, g=g, s=s, hs=hs, p_=p_: e.matmul(
                            out=pw[p_][:, jj * 128:(jj + 1) * 128], lhsT=wqb[s][:, (4 * g + jj) * 128:(4 * g + jj + 1) * 128],
                            rhs=skb[:, hs, :], start=True, stop=True) for jj in range(4)], [R_wqb[s], R_sk], [R_pw[p_]])
                        P.op("act", lambda e, g=g, hs=hs, p_=p_: e.copy(
                            out=Wp[:, 4 * g:4 * g + 4, hs * 128:(hs + 1) * 128],
                            in_=pw[p_][:, :].rearrange("p (j c) -> p j c", c=128)), [R_pw[p_]], [R_Wp])
                done()

            pT = ps(st, "g_pT", [128, 16, 128], BF16)
            psc = [ps(st, f"g_psc{i}", [128, 512], F32) for i in range(4)]
            pTs = ps(st, "g_pTs", [128, 512], F32)
            R_pT, R_psc, R_pTs = Res(), Res(), Res()

            class BS:
                pass

            bsets = []
            for z in range(2):
                B = BS()
                B.h2t = sb(st, f"g_h2t{z}", [128, D], F32)
                B.junk = sb(st, f"g_junk{z}", [128, D], BF16)
                B.hn2T = sb(st, f"g_hn2T{z}", [128, 16, 128], BF16)
                B.sc = sb(st, f"g_sc{z}", [128, 2176], F32)
                B.sc2 = sb(st, f"g_sc2{z}", [128, 2048], F32)
                B.cc = sb(st, f"g_cc{z}", [128, 4352], F32)
                B.cand = B.cc[:, 0:2048].rearrange("p (h c) -> p h c", c=256)
                B.cand2 = B.cc[:, 2048:4096].rearrange("p (h c) -> p h c", c=256)
                B.vals = sb(st, f"g_vals{z}", [128, 16, 16], F32)
                B.idxu = sb(st, f"g_idxu{z}", [128, 16, 16], U32)
                B.idxf = sb(st, f"g_idxf{z}", [128, 16, 16], F32)
                B.ctop = sb(st, f"g_ctop{z}", [128, 8, 16], F32)
                B.cidx = sb(st, f"g_cidx{z}", [128, 8, 16], U32)
                B.cidf = sb(st, f"g_cidf{z}", [128, 8, 16], F32)
                B.sm = [sb(st, f"g_sm{z}_{i}", [128, 8, 16], F32) for i in range(4)]
                B.gi = sb(st, f"g_gi{z}", [128, 3, 128], F32)
                B.slT = sb(st, f"g_slT{z}", [128, 3, 128], F32)
                B.zz = sb(st, f"g_zz{z}", [128, 8], F32)
                B.rz = sb(st, f"g_rz{z}", [128, 8], F32)
                B.ssq = sb(st, f"g_ssq{z}", [128, 1], F32)
                B.rt = sb(st, f"g_rt{z}", [128, 1], F32)
                B.rstd = sb(st, f"g_rstd{z}", [128, 1], F32)
                (B.R_h2t, B.R_hn2T, B.R_gi, B.R_slT, B.R_junk, B.R_sc, B.R_sc2, B.R_cand, B.R_cand2, B.R_vals, B.R_idx,
                 B.R_ctop, B.R_cidx, B.R_sm, B.R_st) = (Res() for _ in range(15))
                B.z = z
                bsets.append(B)

            def tile_ops(i, B):
                z = B.z
                hx, junk, hT, sc, sc2, cand, cand2 = B.h2t, B.junk, B.hn2T, B.sc, B.sc2, B.cand, B.cand2
                vals, idxu, idxf, ctop, cidx, cidf = B.vals, B.idxu, B.idxf, B.ctop, B.cidx, B.cidf
                ssq, rt, rstd, zz, rz = B.ssq, B.rt, B.rstd, B.zz, B.rz
                P.dma("sp", hx[:], s_h2[i * 128:(i + 1) * 128, :], [R_scr["h2"]], [B.R_h2t], f"gh2{z}")
                P.op("act", lambda e: e.activation(out=junk[:], in_=hx[:], func=AF.Square, accum_out=ssq[:]),
                     [B.R_h2t], [B.R_junk, B.R_st])
                P.op("act", lambda e: e.activation(out=rt[:], in_=ssq[:], func=AF.Ln, scale=1.0 / D, bias=EPS),
                     [B.R_st], [B.R_st])
                P.op("act", lambda e: e.activation(out=rstd[:], in_=rt[:], func=AF.Exp, scale=-0.5),
                     [B.R_st], [B.R_st])
                yield "FE1"
                P.op("pool", lambda e: e.tensor_scalar(out=hx[:], in0=hx[:], scalar1=rstd[:, 0:1], scalar2=1.0,
                                                       op0=ALU.mult, op1=ALU.mult), [B.R_h2t, B.R_st], [B.R_h2t])
                P.op("pool", lambda e: e.tensor_tensor(out=junk[:], in0=hx[:], in1=g2bc[:], op=ALU.mult),
                     [B.R_h2t, R_gc], [B.R_junk])
                yield
                P.op("pe", [lambda e, dc=dc: e.transpose(out=pT[:, dc, :], in_=junk[:, dc * 128:(dc + 1) * 128],
                                                         identity=ident[:]) for dc in range(16)],
                     [B.R_junk, R_const], [R_pT])
                P.op("act", lambda e: e.copy(out=hT[:], in_=pT[:]), [R_pT], [B.R_hn2T])
                P.dma("sp", s_hn2T[i // 2][:, :, (i % 2) * 128:(i % 2 + 1) * 128], hT[:], [B.R_hn2T], [R_scr["hn2T"]], f"ghT{z}")
                for blk in range(4):
                    P.op("pe", [lambda e, dc=dc, blk=blk: e.matmul(
                        out=psc[blk][:, :], lhsT=hT[:, dc, :], rhs=Wp[:, dc, blk * 512:(blk + 1) * 512],
                        start=(dc == 0), stop=(dc == 15)) for dc in range(16)], [B.R_hn2T, R_Wp], [R_psc])
                for blk in range(4):
                    P.op("act", lambda e, blk=blk: e.copy(out=sc[:, blk * 512:(blk + 1) * 512], in_=psc[blk][:, :]),
                         [R_psc], [B.R_sc])
                yield "FE2"
                for g in range(16):
                    sg = sc[:, g * 128:(g + 1) * 128]
                    sg2 = sc2[:, g * 128:(g + 1) * 128]
                    P.op("dve", lambda e, g=g, sg=sg: e.max(out=vals[:, g, 0:8], in_=sg), [B.R_sc], [B.R_vals])
                    yield
                    P.op("dve", lambda e, g=g, sg=sg: e.max_index(out=idxu[:, g, 0:8], in_max=vals[:, g, 0:8], in_values=sg),
                         [B.R_sc, B.R_vals], [B.R_idx])
                    P.op("dve", lambda e, g=g, sg=sg, sg2=sg2: e.match_replace(
                        out=sg2, in_to_replace=vals[:, g, 0:8], in_values=sg, imm_value=-1e30), [B.R_sc, B.R_vals], [B.R_sc2])
                    yield
                    P.op("dve", lambda e, g=g, sg2=sg2: e.max(out=vals[:, g, 8:16], in_=sg2), [B.R_sc2], [B.R_vals])
                    yield
                    P.op("dve", lambda e, g=g, sg2=sg2: e.max_index(out=idxu[:, g, 8:16], in_max=vals[:, g, 8:16],
                                                                    in_values=sg2), [B.R_sc2, B.R_vals], [B.R_idx])
                    yield
                yield "SUB"
                P.op("dve", lambda e: e.tensor_copy(out=idxf[:], in_=idxu[:]), [B.R_idx], [B.R_idx])
                v4 = vals[:].rearrange("p (h s) k -> p h s k", s=2)
                i4 = idxf[:].rearrange("p (h s) k -> p h s k", s=2)
                P.op("dve", lambda e: e.tensor_tensor(
                    out=cand.rearrange("p h (a b) -> p h a b", b=16),
                    in0=v4[:, :, 0, :].unsqueeze(3).broadcast_to([128, 8, 16, 16]),
                    in1=v4[:, :, 1, :].unsqueeze(2).broadcast_to([128, 8, 16, 16]), op=ALU.add), [B.R_vals], [B.R_cand])
                yield
                for h in range(8):
                    P.op("dve", lambda e, h=h: e.max(out=ctop[:, h, 0:8], in_=cand[:, h, :]), [B.R_cand], [B.R_ctop])
                    yield
                    P.op("dve", lambda e, h=h: e.max_index(out=cidx[:, h, 0:8], in_max=ctop[:, h, 0:8],
                                                           in_values=cand[:, h, :]), [B.R_cand, B.R_ctop], [B.R_cidx])
                    P.op("dve", lambda e, h=h: e.match_replace(out=cand2[:, h, :], in_to_replace=ctop[:, h, 0:8],
                                                               in_values=cand[:, h, :], imm_value=-1e30),
                         [B.R_cand, B.R_ctop], [B.R_cand2])
                    yield
                    P.op("dve", lambda e, h=h: e.max(out=ctop[:, h, 8:16], in_=cand2[:, h, :]), [B.R_cand2], [B.R_ctop])
                    yield
                    P.op("dve", lambda e, h=h: e.max_index(out=cidx[:, h, 8:16], in_max=ctop[:, h, 8:16],
                                                           in_values=cand2[:, h, :]), [B.R_cand2, B.R_ctop], [B.R_cidx])
                    yield
                P.op("dve", lambda e: e.tensor_copy(out=cidf[:], in_=cidx[:]), [B.R_cidx], [B.R_cidx])
                yield "CAND"
                dd, ex, k1f, k2f = B.sm
                gv = B.gi
                gate3 = gv[:, 0, :].rearrange("p (h k) -> p h k", k=16)
                i1f3 = gv[:, 1, :].rearrange("p (h k) -> p h k", k=16)
                i2f3 = gv[:, 2, :].rearrange("p (h k) -> p h k", k=16)
                P.op("dve", lambda e: e.tensor_tensor(out=dd[:], in0=ctop[:], in1=ctop[:, :, 0:1].broadcast_to([128, 8, 16]),
                                                      op=ALU.subtract), [B.R_ctop], [B.R_sm])
                P.op("act", lambda e: e.activation(out=ex[:], in_=dd[:], func=AF.Exp), [B.R_sm], [B.R_sm])
                yield
                ge = B.cc[:, 0:2176].rearrange("p (h j k) -> p h j k", h=8, j=16)
                eq = sc2[:, :].rearrange("p (h j k) -> p h j k", h=8, j=16)
                P.op("dve", lambda e: e.tensor_tensor(
                    out=ge, in0=cidf[:].unsqueeze(3).broadcast_to([128, 8, 16, 17]),
                    in1=thr17.unsqueeze(1).unsqueeze(1).broadcast_to([128, 8, 16, 17]), op=ALU.is_ge),
                    [B.R_cidx, R_gc], [B.R_cand, B.R_cand2])
                yield
                P.op("dve", lambda e: e.tensor_reduce(out=zz[:], in_=ex[:], axis=AX.X, op=ALU.add), [B.R_sm], [B.R_sm])
                yield
                P.op("dve", lambda e: e.tensor_reduce(out=k1f[:], in_=ge[:, :, :, 1:16], axis=AX.X, op=ALU.add),
                     [B.R_cand, B.R_cand2], [B.R_sm])
                yield
                P.op("dve", lambda e: e.reciprocal(out=rz[:], in_=zz[:]), [B.R_sm], [B.R_sm])
                yield
                P.op("dve", lambda e: e.tensor_tensor(out=eq, in0=ge[:, :, :, 0:16], in1=ge[:, :, :, 1:17],
                                                      op=ALU.subtract), [B.R_cand, B.R_cand2], [B.R_sc2])
                yield
                P.op("dve", lambda e: e.tensor_tensor(
                    out=gate3, in0=ex[:], in1=rz[:].unsqueeze(2).broadcast_to([128, 8, 16]), op=ALU.mult),
                    [B.R_sm], [B.R_gi])
                yield
                P.op("dve", lambda e: e.tensor_tensor(
                    out=eq, in0=eq, in1=i4[:, :, 0, :].unsqueeze(2).broadcast_to([128, 8, 16, 16]), op=ALU.mult),
                    [B.R_sc2, B.R_idx], [B.R_sc2])
                yield
                P.op("dve", lambda e: e.scalar_tensor_tensor(out=k2f[:], in0=k1f[:], scalar=-16.0, in1=cidf[:],
                                                             op0=ALU.mult, op1=ALU.add), [B.R_sm, B.R_cidx], [B.R_sm])
                yield
                P.op("dve", lambda e: e.tensor_reduce(out=i1f3, in_=eq, axis=AX.X, op=ALU.add), [B.R_sc2], [B.R_gi])
                yield
                P.op("dve", lambda e: e.tensor_tensor(
                    out=eq, in0=k2f[:].unsqueeze(3).broadcast_to([128, 8, 16, 16]),
                    in1=iota16.unsqueeze(1).unsqueeze(1).broadcast_to([128, 8, 16, 16]), op=ALU.is_equal),
                    [B.R_sm, R_gc], [B.R_sc2])
                yield
                P.op("dve", lambda e: e.tensor_tensor(
                    out=eq, in0=eq, in1=i4[:, :, 1, :].unsqueeze(2).broadcast_to([128, 8, 16, 16]), op=ALU.mult),
                    [B.R_sc2, B.R_idx], [B.R_sc2])
                yield
                P.op("dve", lambda e: e.tensor_reduce(out=i2f3, in_=eq, axis=AX.X, op=ALU.add), [B.R_sc2], [B.R_gi])
                yield
                P.op("pe", [lambda e, k_=k_: e.transpose(out=pTs[:, k_ * 128:(k_ + 1) * 128], in_=gv[:, k_, :],
                                                         identity=ident_f[:]) for k_ in range(3)],
                     [B.R_gi, R_const], [R_pTs])
                P.op("act", lambda e: e.copy(out=B.slT[:].rearrange("p k t -> p (k t)"), in_=pTs[:, 0:384]),
                     [R_pTs], [B.R_slT])
                P.dma("sp", s_slot[i], B.slT[:].rearrange("p k t -> p (k t)"), [B.R_slT], [R_scr["slot"]], f"gsl{z}")
                yield

            def run_until(gen, tag):
                for v in gen:
                    if v == tag:
                        return
                    yield

            def lane(z):
                gens = [tile_ops(i, bsets[z]) for i in range(z, 16, 2)]
                yield from run_until(gens[0], "FE2")
                for k_, cur in enumerate(gens):
                    nxt = gens[k_ + 1] if k_ + 1 < len(gens) else None
                    yield from run_until(cur, "SUB")
                    if nxt is not None:
                        yield from run_until(nxt, "FE1")
                    yield from run_until(cur, "CAND")
                    if nxt is not None:
                        yield from run_until(nxt, "FE2")
                    yield from run_until(cur, "END")

            alive = [lane(0), lane(1)]
            while alive:
                for gen in list(alive):
                    if next(gen, "end") == "end":
                        alive.remove(gen)
            done()
        if stop_after == "G0":
            return nc

        with ExitStack() as st:
            Gb = [sb(st, f"h_G{i}", [128, 128, 256], BF16) for i in range(2)]
            hnb = sb(st, "h_hnb", [128, 16, 256], BF16)
            NU, NV = 4, 7
            ubuf = [sb(st, f"h_ub{i}", [128, 16, 128], BF16) for i in range(NU)]
            vbuf = [sb(st, f"h_vb{i}", [128, 1024], BF16) for i in range(NV)]
            slT = [sb(st, f"h_slT{i}", [128, 3, 128], F32) for i in range(4)]
            LR = [sb(st, f"h_LR{i}", [128, 8, 2, 128], BF16) for i in range(2)]
            io128 = sb(st, "h_io", [128, 128], F32)
            gl = [sb(st, f"h_gl{i}", [128, 256], BF16) for i in range(2)]
            h2t = [sb(st, f"h_h2t{i}", [128, D], F32) for i in range(2)]
            gfbc = sb(st, "h_gfbc", [128, D], F32)
            ssq = sb(st, "h_ssq", [128, 1], F32)
            rt = sb(st, "h_rt", [128, 1], F32)
            rstd = sb(st, "h_rstd", [128, 1], F32)
            pacc = [ps(st, f"h_pacc{i}", [128, 512], F32) for i in range(4)]
            psS = [ps(st, f"h_psS{i}", [128, 512], F32) for i in range(2)]
            pG = [ps(st, f"h_pG{i}", [128, 512], F32) for i in range(2)]
            R_ub = [Res() for _ in range(NU)]
            R_vb = [Res() for _ in range(NV)]
            R_slT = [Res() for _ in range(4)]
            R_LR, R_gl, R_h2t, R_psS, R_pG, R_G = ([Res(), Res()] for _ in range(6))
            R_pacc = [Res() for _ in range(4)]
            R_hnb, R_gc, R_st = (Res() for _ in range(3))
            junk = LR[0][:].rearrange("p a b c -> p (a b c)")
            P.dma("sp", gfbc[:], gf.partition_broadcast(128), [], [R_gc], "hc")
            P.dma("sp", io128[:], peer_c[:, 33:161], [], [R_gc], "hc")
            cn = {"u": 0, "v": 0, "q": 0, "g": 0, "s": 0}

            def gbuild(b):
                G = Gb[b % 2]
                RG = R_G[b % 2]
                for tt in range(2):
                    ti = 2 * b + tt
                    sl = (2 * b + tt) % 4
                    P.dma("pool", slT[sl][:].rearrange("p k t -> p (k t)"), s_slot[ti], [R_scr["slot"]], [R_slT[sl]], f"hsl{sl}")
                yield
                for q in range(32):
                    tt = q // 16
                    sl = (2 * b + tt) % 4
                    t0 = (q % 16) * 8
                    lr = cn["q"] % 2
                    cn["q"] += 1
                    fns = []
                    for t in range(8):
                        fns.append(lambda e, t=t, sl=sl, t0=t0, lr=lr: e.tensor_scalar(
                            out=LR[lr][:, t, 0, :], in0=io128[:], scalar1=slT[sl][:, 1, t0 + t:t0 + t + 1],
                            scalar2=slT[sl][:, 0, t0 + t:t0 + t + 1], op0=ALU.is_equal, op1=ALU.mult))
                        fns.append(lambda e, t=t, sl=sl, t0=t0, lr=lr: e.tensor_scalar(
                            out=LR[lr][:, t, 1, :], in0=io128[:], scalar1=slT[sl][:, 2, t0 + t:t0 + t + 1],
                            scalar2=None, op0=ALU.is_equal))
                    P.op("dve", fns, [R_slT[sl], R_gc], [R_LR[lr]])
                    for g in range(2):
                        pg = cn["g"] % 2
                        cn["g"] += 1
                        P.op("pe", [lambda e, jj=jj, g=g, lr=lr, pg=pg: e.matmul(
                            out=pG[pg][:, jj * 128:(jj + 1) * 128], lhsT=LR[lr][:, 4 * g + jj, 0, :],
                            rhs=LR[lr][:, 4 * g + jj, 1, :], start=True, stop=True) for jj in range(4)],
                            [R_LR[lr]], [R_pG[pg]])
                        c0 = tt * 128 + t0 + 4 * g
                        P.op("act", lambda e, pg=pg, c0=c0, G=G: e.copy(
                            out=G[:, :, c0:c0 + 4], in_=pG[pg][:, :].rearrange("p (t i) -> p i t", i=128)),
                            [R_pG[pg]], [RG])
                    yield

            def load_hnb(b):
                P.dma("pool", hnb[:], s_hn2T[b], [R_scr["hn2T"]], [R_hnb], "hhn")

            pend_epi = []

            def epilogue_g1(b):
                for tt in range(2):
                    ti = 2 * b + tt
                    hx = h2t[tt]
                    P.op("act", lambda e, hx=hx: e.activation(out=junk, in_=hx[:], func=AF.Square, accum_out=ssq[:]),
                         [R_h2t[tt]], [R_LR[0], R_st])
                    P.op("act", lambda e: e.activation(out=rt[:], in_=ssq[:], func=AF.Sqrt, scale=1.0 / D, bias=EPS),
                         [R_st], [R_st])
                    P.op("dve", lambda e: e.reciprocal(out=rstd[:], in_=rt[:]), [R_st], [R_st])
                    P.op("dve", lambda e, hx=hx: e.scalar_tensor_tensor(
                        out=hx[:], in0=hx[:], scalar=rstd[:, 0:1], in1=gfbc[:], op0=ALU.mult, op1=ALU.mult),
                        [R_h2t[tt], R_st, R_gc], [R_h2t[tt]])
                    P.dma("pool", out_d[ti * 128:(ti + 1) * 128, :], hx[:], [R_h2t[tt]], [Res()], f"hout{tt}")

            for _ in gbuild(0):
                pass
            for b in range(8):
                G = Gb[b % 2]
                RG = R_G[b % 2]
                gnext = gbuild(b + 1) if b + 1 < 8 else iter(())
                if b == 0:
                    load_hnb(0)

                def load_u(i2):
                    r = cn["u"] % NU
                    cn["u"] += 1
                    P.dma("sp", ubuf[r][:].rearrange("p c i -> p (c i)"), s_uT[i2], [R_scr["uT"]], [R_ub[r]], f"hub{r}")
                    return r

                ring = [load_u(i2) for i2 in range(NU - 1)]
                for i2 in range(128):
                    if i2 + NU - 1 < 128:
                        ring.append(load_u(i2 + NU - 1))
                    r = ring[i2]
                    s_ = cn["s"] % 2
                    cn["s"] += 1
                    P.op("pe", [lambda e, dc=dc, r=r, s_=s_: e.matmul(
                        out=psS[s_][:, 0:256], lhsT=ubuf[r][:, dc, :], rhs=hnb[:, dc, :],
                        start=(dc == 0), stop=(dc == 15)) for dc in range(16)], [R_ub[r], R_hnb], [R_psS[s_]])
                    P.op("act", lambda e, s_=s_: e.activation(out=gl[s_][:], in_=psS[s_][:, 0:256],
                                                              func=AF.Gelu_apprx_tanh), [R_psS[s_]], [R_gl[s_]])
                    P.op("dve", lambda e, s_=s_, i2=i2, G=G: e.tensor_tensor(out=G[:, i2, :], in0=gl[s_][:], in1=G[:, i2, :],
                                                                             op=ALU.mult), [R_gl[s_], RG], [RG])
                    if i2 == 3 and pend_epi:
                        epilogue_g1(pend_epi.pop(0))
                if b + 1 < 8:
                    load_hnb(b + 1)
                for tt in range(2):
                    ti = 2 * b + tt
                    P.dma("pool", h2t[tt][:], s_h2[ti * 128:(ti + 1) * 128, :], [R_scr["h2"]], [R_h2t[tt]], f"hh2{tt}")

                def load_v(i2, half):
                    r = cn["v"] % NV
                    cn["v"] += 1
                    P.dma("sp", vbuf[r][:], s_v2[i2][:, half * 1024:(half + 1) * 1024], [R_scr["v2"]], [R_vb[r]], f"hvb{r}")
                    return r

                for half in range(2):
                    ring = [load_v(i2, half) for i2 in range(NV - 1)]
                    for i2 in range(128):
                        if i2 + NV - 1 < 128:
                            ring.append(load_v(i2 + NV - 1, half))
                        r = ring[i2]
                        P.op("pe", [lambda e, tt=tt, db=db, r=r, i2=i2, G=G: e.matmul(
                            out=pacc[tt * 2 + db][:, :], lhsT=G[:, i2, tt * 128:(tt + 1) * 128],
                            rhs=vbuf[r][:, db * 512:(db + 1) * 512], start=(i2 == 0), stop=(i2 == 127))
                            for tt in range(2) for db in range(2)], [R_vb[r], RG], R_pacc)
                        if i2 % 6 == 2:
                            next(gnext, None)
                    for tt in range(2):
                        for db in range(2):
                            c0 = half * 1024 + db * 512
                            P.op("dve", lambda e, tt=tt, db=db, c0=c0: e.tensor_tensor(
                                out=h2t[tt][:, c0:c0 + 512], in0=pacc[tt * 2 + db][:, :], in1=h2t[tt][:, c0:c0 + 512],
                                op=ALU.add), [R_pacc[tt * 2 + db], R_h2t[tt]], [R_h2t[tt]])
                for _ in gnext:
                    pass
                pend_epi.append(b)
            epilogue_g1(pend_epi.pop(0))
            done()
    return nc


def _host_inputs(inp):
    f = lambda a: np.ascontiguousarray(a, dtype=np.float32)
    w_in = inp["w_in"][0]
    kr = w_in[:, 4096:4160]
    shared = {
        "meta": f(inp["meta_tokens"]),
        "g1": f(inp["norm1_g"][0][None]),
        "g2": f(inp["norm2_g"][0][None]),
        "gf": f(inp["final_norm_g"][None]),
        "w_in": f(w_in),
        "w_kr2": f(np.concatenate([kr, kr[:, 32:], kr[:, :32]], axis=1)),
        "gq": f(inp["mla_q_norm_g"][0].reshape(4, 128).T),
        "gkv": f(inp["mla_kv_norm_g"][0].reshape(4, 128).T),
        "w_ukv": f(inp["mla_w_ukv"][0]),
        "ident": np.eye(128, dtype=np.float32),
        "w_na": f(inp["w_na_branch"][0]),
        "w_mla": f(inp["w_mla_branch"][0]),
        "w_out": f(inp["w_out"][0]),
        "puT": f(inp["peer_u"][0].reshape(128, 128, 16, 128).transpose(1, 3, 2, 0).reshape(128, 128, D)),
        "pv2": f(inp["peer_v"][0].reshape(128, 128, D).transpose(1, 0, 2)),
    }
    wq = inp["mla_w_uq"][0].reshape(512, 16, 192)
    shared["w_uqh"] = f(np.concatenate([wq, wq[:, :, 160:], wq[:, :, 128:160]], axis=2).transpose(1, 0, 2))
    pos = np.concatenate([np.arange(NM, T), np.arange(NM)]).astype(np.float32)
    inv_freq = (10000.0 ** (-np.arange(0, 64, 2, dtype=np.float32) / 64)).astype(np.float32)
    ang = pos[None, :] * inv_freq[:, None]
    cos, sin = np.cos(ang).astype(np.float32), np.sin(ang).astype(np.float32)
    shared["ropeCS"] = f(np.stack([np.concatenate([cos, cos], 0), np.concatenate([-sin, sin], 0)]))
    rb = inp["na_rel_bias"][0]
    mb = inp["na_meta_bias"][0]
    rows = 32

    def rs(r):
        return int(np.clip(r - 4, 0, rows - 8))

    def cs(c):
        return np.clip(c - 8, 0, 64 - 16)

    def pattern(i, kt):
        kk = np.arange(128)
        kr_ = 2 * kt + kk // 64
        kc_ = kk % 64
        qr_ = 2 * i + kk // 64
        qc_ = kk % 64
        dr = kr_[:, None] - qr_[None, :]
        dc = kc_[:, None] - qc_[None, :]
        rs_q = np.array([rs(r) for r in qr_])
        cs_q = cs(qc_)
        valid = ((kr_[:, None] >= rs_q[None, :]) & (kr_[:, None] < rs_q[None, :] + 8)
                 & (kc_[:, None] >= cs_q[None, :]) & (kc_[:, None] < cs_q[None, :] + 16))
        ro = np.clip(dr + 7, 0, 14)
        co = np.clip(dc + 15, 0, 30)
        g = rb[:, ro, co]
        g = np.where(valid[None], g, np.float32(NEG)).astype(np.float32)
        return g.transpose(1, 0, 2)

    def halves(p):
        o = []
        for hp in range(2):
            hs = [8 * hp + 2 * pp + e for e in range(2) for pp in range(4)]
            o.append(p[:, hs, :].reshape(p.shape[0], 1024))
        return o

    tI = [halves(pattern(6, 6 + dt)) for dt in range(-2, 3)]
    shared["tabI"] = f(np.stack([np.stack([t[hp] for t in tI]) for hp in range(2)]))
    bl = []
    for i, k0 in ((0, 0), (1, 0), (14, 12), (15, 12)):
        for kt in range(k0, k0 + 4):
            bl.append(halves(pattern(i, kt)))
    shared["tabB"] = f(np.stack([np.stack([t[hp] for t in bl]) for hp in range(2)]))
    pm = np.broadcast_to(mb.T[:, :, None], (16, 16, 128))
    shared["tabM"] = f(np.stack(halves(pm)))
    shared["peer_c"] = f(np.broadcast_to(np.concatenate([np.arange(16), 16 * np.arange(17), np.arange(128)])[None, :], (128, 161)))
    shared["w_qT"] = f(inp["peer_w_q"][0].T.reshape(16, 128, D))
    shared["subkT"] = f(inp["peer_sub_keys"][0].reshape(16, 128, 128).transpose(0, 2, 1))
    return shared


_CACHE = {}


def kernel(**inp):
    shared = _host_inputs(inp)
    if "nc" not in _CACHE:
        _CACHE["nc"] = build()
    nc = _CACHE["nc"]
    in_maps = []
    for b in range(NCORES):
        m = dict(shared)
        m["x"] = np.ascontiguousarray(inp["x"][b], dtype=np.float32)
        in_maps.append(m)
    res = run_bass_kernel_spmd(nc, in_maps, core_ids=list(range(NCORES)))
    return np.stack([np.asarray(r["out"], dtype=np.float32) for r in res.results], axis=0)
```

```python
import numpy as np
from contextlib import ExitStack
import concourse.bass as bass
import concourse.mybir as mybir
from concourse.bass_utils import run_bass_kernel_spmd

F32 = mybir.dt.float32
BF16 = mybir.dt.bfloat16
I32 = mybir.dt.int32
U32 = mybir.dt.uint32
AF = mybir.ActivationFunctionType
ALU = mybir.AluOpType
AX = mybir.AxisListType

D = 2048
S = 2048
NM = 16
T = S + NM
EPS = 1e-6
NCORES = 8
QB = [(i * 512, 512) for i in range(4)]
TB = QB + [(2048, 16)]
NA_SCALE = 64 ** -0.5
MLA_SCALE = 192 ** -0.5
NEG = -30000.0


class Res:
    __slots__ = ("lw", "rd")

    def __init__(self):
        self.lw = None
        self.rd = {}


class Prog:
    ENG = ("pe", "act", "dve", "pool", "sp")

    def __init__(self, nc, stack):
        self.nc = nc
        self.stack = stack
        self.sem = {}
        self.val = {}
        self.seen = {e: {} for e in self.ENG}
        self.q = {e: [] for e in self.ENG}
        self.n = 0

    def _sem(self, key):
        if key not in self.sem:
            self.sem[key] = self.stack.enter_context(self.nc.semaphore("s_" + key))
            self.val[key] = 0

    def _waits(self, eng, reads, writes):
        need = {}

        def add(k, v):
            if need.get(k, 0) < v:
                need[k] = v

        for r in reads:
            if r.lw is not None:
                add(*r.lw)
        for w in writes:
            if w.lw is not None:
                add(*w.lw)
            for k, v in w.rd.items():
                add(k, v)
        out = []
        for k, v in need.items():
            if k == "pe" and eng == "pe":
                continue
            if self.seen[eng].get(k, 0) < v:
                self.seen[eng][k] = v
                out.append((k, v))
        return out

    def op(self, eng, fns, reads=(), writes=(), semkey=None, inc=1):
        if not isinstance(fns, (list, tuple)):
            fns = [fns]
        waits = self._waits(eng, reads, writes)
        key = semkey or eng
        self._sem(key)
        self.val[key] += inc
        ev = (key, self.val[key])
        for r in reads:
            if r.rd.get(key, 0) < ev[1]:
                r.rd[key] = ev[1]
        for w in writes:
            w.lw = ev
            w.rd = {}
        self.q[eng].append((waits, fns, key, inc))
        self.n += len(fns)
        return ev

    def dma(self, eng, out, in_, reads, writes, semkey, **kw):
        return self.op(eng, lambda e: e.dma_start(out=out, in_=in_, **kw), reads, writes, semkey=semkey, inc=16)

    def flush(self, drain=True):
        nc = self.nc
        if drain:
            w = []
            for k, v in self.val.items():
                if k not in self.ENG and self.seen["sp"].get(k, 0) < v:
                    self.seen["sp"][k] = v
                    w.append((k, v))
            if w:
                self.q["sp"].append((w, [], None, 0))
        sem = self.sem
        with nc.Block() as block:
            for ename, deco in (("pe", block.tensor), ("act", block.scalar), ("dve", block.vector),
                                ("pool", block.gpsimd), ("sp", block.sync)):
                items = self.q[ename]

                def body(e, items=items):
                    for waits, fns, key, inc in items:
                        for k, v in waits:
                            e.wait_ge(sem[k], v)
                        last = None
                        for fn in fns:
                            last = fn(e)
                        if last is not None:
                            last.then_inc(sem[key], inc)

                deco(body)
        self.q = {e: [] for e in self.ENG}


class Ctx:
    pass


def _sb(nc, st, name, shape, dt):
    return st.enter_context(nc.sbuf_tensor(name, list(shape), dt))


def _ps(nc, st, name, shape, dt):
    return st.enter_context(nc.psum_tensor(name, list(shape), dt))


def build(debug=None, stop_after=None):
    debug = debug or ()
    nc = bass.Bass("TRN2", target_bir_lowering=False)

    def din(name, shape, dt=F32):
        return nc.dram_tensor(name, list(shape), dt, kind="ExternalInput").ap()

    def dscr(name, shape, dt):
        kind = "ExternalOutput" if name in debug else "Internal"
        return nc.dram_tensor(name, list(shape), dt, kind=kind).ap()

    x = din("x", [S, D])
    meta = din("meta", [NM, D])
    g1 = din("g1", [1, D])
    g2 = din("g2", [1, D])
    gf = din("gf", [1, D])
    w_in = din("w_in", [D, 8256])
    w_kr2 = din("w_kr2", [D, 128])
    gq = din("gq", [128, 4])
    gkv = din("gkv", [128, 4])
    w_uqh = din("w_uqh", [16, 512, 256])
    w_ukv = din("w_ukv", [512, 4096])
    ropeCS = din("ropeCS", [2, 64, T])
    ident_d = din("ident", [128, 128])
    tabI = din("tabI", [2, 5, 128, 1024])
    tabB = din("tabB", [2, 16, 128, 1024])
    tabM = din("tabM", [2, 16, 1024])
    w_na = din("w_na", [1024, D])
    w_mla = din("w_mla", [D, D])
    w_out = din("w_out", [D, D])
    PEER_ON = stop_after in (None, "G0", "G1")
    if PEER_ON:
        w_qT = din("w_qT", [16, 128, D])
        subkT = din("subkT", [16, 128, 128])
        puT = din("puT", [128, 128, D])
        pv2 = din("pv2", [128, 128, D])
        peer_c = din("peer_c", [128, 161])
    out_d = nc.dram_tensor("out", [S, D], F32, kind="ExternalOutput").ap()

    s_naq = dscr("s_naq", [8, 128, S], BF16)
    s_nak = dscr("s_nak", [8, 128, T], BF16)
    s_nav = dscr("s_nav", [T, 1024], BF16)
    s_ga = dscr("s_ga", [16, 128, S], BF16)
    s_gb = dscr("s_gb", [16, 128, S], BF16)
    s_omla = dscr("s_omla", [16, 128, S], BF16)
    s_ona = dscr("s_ona", [8, 128, S], BF16)
    s_h2 = dscr("s_h2", [S, D], F32)
    s_merged = dscr("s_merged", [16, 128, S], BF16)
    s_hn2T = dscr("s_hn2T", [8, 128, 16, 256], BF16)
    s_slot = dscr("s_slot", [16, 128, 384], F32)
    s_uT = dscr("s_uT", [128, 128, D], BF16)
    s_v2 = dscr("s_v2", [128, 128, D], BF16)
    d_cqn = dscr("d_cqn", [128, 4, S], BF16) if "d_cqn" in debug else None
    d_ckvn = dscr("d_ckvn", [128, 4, T], BF16) if "d_ckvn" in debug else None
    d_krope = dscr("d_krope", [128, T], BF16) if "d_krope" in debug else None
    d_merged = dscr("d_merged", [128, 16, S], BF16) if "d_merged" in debug else None
    R_scr = {k: Res() for k in ("naq", "nak", "nav", "ga", "gb", "omla", "ona", "h2", "dbg", "merged", "hn2T", "slot", "uT", "v2")}

    with ExitStack() as top:
        P = Prog(nc, top)
        sb = lambda st, name, shape, dt: _sb(nc, st, name, shape, dt)
        ps = lambda st, name, shape, dt: _ps(nc, st, name, shape, dt)

        ident_f = sb(top, "ident_f", [128, 128], F32)
        ident = sb(top, "identb", [128, 128], BF16)
        ones_bf = sb(top, "ones_bf", [128, 128], BF16)
        ones_f = sb(top, "ones_f", [128, 128], F32)
        R_const = Res()
        P.dma("sp", ident_f[:], ident_d, [], [R_const], "c0")
        P.op("dve", lambda e: e.tensor_copy(out=ident[:], in_=ident_f[:]), [R_const], [R_const])
        P.op("dve", lambda e: e.memset(ones_bf[:], 1.0), [], [R_const])
        P.op("dve", lambda e: e.memset(ones_f[:], 1.0), [], [R_const])

        def done():
            P.flush()

        pcb = {}

        def pc_alloc(st, tag):
            pcb["stg"] = [sb(st, f"pc_stg{tag}{i}", [128, D], F32) for i in range(2)]
            pcb["cst"] = [sb(st, f"pc_cst{tag}{i}", [128, D], BF16) for i in range(2)]
            pcb["Rs"] = [Res(), Res()]
            pcb["Rc"] = [Res(), Res()]
            pcb["fresh"] = True

        def _pc_gen():
            steps = [(src, dst, key, i2) for i2 in range(128) for src, dst, key in ((puT, s_uT, "uT"), (pv2, s_v2, "v2"))]

            def load(k):
                src, dst, key, i2 = steps[k]
                s = k % 2
                P.dma("pool", pcb["stg"][s][:], src[i2], [], [pcb["Rs"][s]], f"pcl{s}")

            for k in range(len(steps)):
                src, dst, key, i2 = steps[k]
                s = k % 2
                if k == 0 or pcb.get("fresh"):
                    pcb["fresh"] = False
                    load(k)
                stg, cst, R_pcs, R_pcc = pcb["stg"], pcb["cst"], pcb["Rs"], pcb["Rc"]
                if k + 1 < len(steps):
                    load(k + 1)
                P.op("pool", lambda e, s=s, stg=stg, cst=cst: e.tensor_copy(out=cst[s][:], in_=stg[s][:]),
                     [R_pcs[s]], [R_pcc[s]])
                P.dma("pool", dst[i2], cst[s][:], [R_pcc[s]], [R_scr[key]], f"pcs{s}")
                yield

        pc_it = _pc_gen() if PEER_ON else iter(())

        def pc_advance(n):
            for _ in range(n):
                if next(pc_it, "end") == "end":
                    break

        with ExitStack() as stAB:
            cqnT = sb(stAB, "cqnT", [128, 4, S], BF16)
            ckvnT = sb(stAB, "ckvnT", [128, 4, T], BF16)
            kropeT = sb(stAB, "kropeT", [128, T], BF16)
            ropeC = sb(stAB, "ropeC", [64, T], F32)
            ropeS = sb(stAB, "ropeS", [64, T], F32)
            R_cqn, R_ckvn, R_krope, R_rope = Res(), Res(), Res(), Res()
            P.dma("sp", ropeC[:], ropeCS[0], [], [R_rope], "c1")
            P.dma("sp", ropeS[:], ropeCS[1], [], [R_rope], "c1")
            P.op("dve", lambda e: e.memset(kropeT[:], 0.0), [], [R_krope])

            with ExitStack() as stB:
                hnT = sb(stB, "hnT", [128, 16, T], BF16)
                R_hnT = Res()
                with ExitStack() as st:
                    xt = [sb(st, f"xt{i}", [128, D], F32) for i in range(2)]
                    hn = [sb(st, f"hn{i}", [128, D], BF16) for i in range(2)]
                    junk = sb(st, "junkA", [128, D], BF16)
                    g1bc = sb(st, "g1bc", [128, D], F32)
                    ssq = [sb(st, f"ssq{i}", [128, 1], F32) for i in range(2)]
                    rt = [sb(st, f"rt{i}", [128, 1], F32) for i in range(2)]
                    rstd = [sb(st, f"rstd{i}", [128, 1], F32) for i in range(2)]
                    pT = [ps(st, f"pT{i}", [128, 16, 128], BF16) for i in range(2)]
                    R_xt, R_hn, R_ss, R_rt, R_rstd, R_pT = ([Res(), Res()] for _ in range(6))
                    R_junk, R_g = Res(), Res()
                    P.dma("sp", g1bc[:], g1.partition_broadcast(128), [], [R_g], "c3")
                    pend_a = []

                    def flush_copy_a():
                        s, rows, col0 = pend_a.pop(0)
                        P.op("act", lambda e: e.copy(out=hnT[:, :, col0:col0 + rows], in_=pT[s][:, :, :rows]),
                             [R_pT[s]], [R_hnT])

                    for i in range(17):
                        s = i % 2
                        rows = 128 if i < 16 else NM
                        src = x[i * 128:(i + 1) * 128, :] if i < 16 else meta
                        col0 = i * 128
                        P.dma("sp", xt[s][:rows, :], src, [], [R_xt[s]], f"xt{s}")
                        P.op("act", lambda e, s=s, rows=rows: e.activation(
                            out=junk[:rows, :], in_=xt[s][:rows, :], func=AF.Square, accum_out=ssq[s][:rows, :]),
                            [R_xt[s]], [R_junk, R_ss[s]])
                        P.op("act", lambda e, s=s, rows=rows: e.activation(
                            out=rt[s][:rows, :], in_=ssq[s][:rows, :], func=AF.Sqrt, scale=1.0 / D, bias=EPS),
                            [R_ss[s]], [R_rt[s]])
                        if pend_a:
                            flush_copy_a()
                        P.op("dve", lambda e, s=s, rows=rows: e.reciprocal(out=rstd[s][:rows, :], in_=rt[s][:rows, :]),
                             [R_rt[s]], [R_rstd[s]])
                        P.op("dve", lambda e, s=s, rows=rows: e.scalar_tensor_tensor(
                            out=hn[s][:rows, :], in0=xt[s][:rows, :], scalar=rstd[s][:rows, 0:1], in1=g1bc[:rows, :],
                            op0=ALU.mult, op1=ALU.mult), [R_xt[s], R_rstd[s], R_g], [R_hn[s]])
                        P.op("pe", [lambda e, s=s, rows=rows, dc=dc: e.transpose(
                            out=pT[s][:, dc, :rows], in_=hn[s][:rows, dc * 128:(dc + 1) * 128],
                            identity=ident[:rows, :rows]) for dc in range(16)], [R_hn[s], R_const], [R_pT[s]])
                        pend_a.append((s, rows, col0))
                    while pend_a:
                        flush_copy_a()
                    done()

                if stop_after == "A":
                    return nc
                with ExitStack() as st:
                    NW = 128
                    wst = [sb(st, f"wst{i}", [128, 16, NW], F32) for i in range(2)]
                    wbf = [sb(st, f"wbf{i}", [128, 16, NW], BF16) for i in range(2)]
                    R_wst, R_wbf = [Res(), Res()], [Res(), Res()]
                    acc = [ps(st, f"acc{i}", [128, 512], F32) for i in range(4)]
                    R_acc = [Res() for _ in range(4)]
                    outt = [sb(st, f"outt{i}", [128, T], BF16) for i in range(2)]
                    R_outt = [Res() for _ in range(2)]
                    cT = sb(st, "cT", [128, 4, T], F32)
                    R_cT = Res()
                    sqt = sb(st, "sqt", [128, 4, 512], F32)
                    rtt = sb(st, "rtt", [128, 512], F32)
                    rstdbc = sb(st, "rstdbc", [128, 512], F32)
                    R_sqt, R_rtt, R_rbc = Res(), Res(), Res()
                    gcol = sb(st, "gcol", [128, 8], F32)
                    R_gcol = Res()
                    t1 = sb(st, "t1", [64, 512], F32)
                    t2 = sb(st, "t2", [64, 512], F32)
                    R_t1, R_t2 = Res(), Res()
                    vout = [sb(st, f"vout{i}", [128, 4, 128], BF16) for i in range(2)]
                    R_vout = [Res() for _ in range(2)]
                    pTv = [ps(st, f"pTv{i}", [128, 512], BF16) for i in range(2)]
                    R_pTv = [Res(), Res()]
                    blocks_nav = TB
                    P.dma("sp", gcol[:, 0:4], gq, [], [R_gcol], "c4")
                    P.dma("sp", gcol[:, 4:8], gkv, [], [R_gcol], "c4")

                    groups = []
                    for j in range(8):
                        groups.append(("naq", w_in[:, j * 128:(j + 1) * 128], 128, QB, j))
                    for j in range(8):
                        groups.append(("nak", w_in[:, 1024 + j * 128:1024 + (j + 1) * 128], 128, TB, j))
                    for j in range(4):
                        groups.append(("cq", w_in[:, 3072 + j * 128:3072 + (j + 1) * 128], 128, QB, j))
                    for j in range(16):
                        groups.append(("ga", w_in[:, 4160 + j * 128:4160 + (j + 1) * 128], 128, QB, j))
                    for j in range(4):
                        groups.append(("ckv", w_in[:, 3584 + j * 128:3584 + (j + 1) * 128], 128, TB, j))
                    for j in range(16):
                        groups.append(("gb", w_in[:, 6208 + j * 128:6208 + (j + 1) * 128], 128, QB, j))
                    groups.append(("kr", w_kr2, 128, TB, 0))
                    for j in range(8):
                        groups.append(("nav", w_in[:, 2048 + j * 128:2048 + (j + 1) * 128], 128, None, j))
                    norm_it = [iter(())]

                    def load_group(gi):
                        kind, wap, n, blocks, j = groups[gi]
                        s = gi % 2
                        P.dma("sp", wst[s][:, :, :n], wap.rearrange("(c p) f -> p c f", p=128), [], [R_wst[s]], f"wst{s}")
                        P.op("pool", lambda e, s=s, n=n: e.tensor_copy(out=wbf[s][:, :, :n], in_=wst[s][:, :, :n]),
                             [R_wst[s]], [R_wbf[s]])

                    cnt = {"acc": 0, "outt": 0, "vout": 0}

                    def next_acc():
                        a = cnt["acc"] % 4
                        cnt["acc"] += 1
                        return a

                    def proj_fm(s, f0, m, c0, n, a):
                        P.op("pe", [lambda e, dc=dc: e.matmul(
                            out=acc[a][:m, :n], lhsT=wbf[s][:, dc, f0:f0 + m], rhs=hnT[:, dc, c0:c0 + n],
                            start=(dc == 0), stop=(dc == 15)) for dc in range(16)],
                            [R_wbf[s], R_hnT], [R_acc[a]])

                    def c_norm(ncols, blocks, gofs, dstT, R_dst):
                        for (c0, n) in blocks:
                            yield
                            P.op("act", lambda e, c0=c0, n=n: e.activation(
                                out=sqt[:, :, :n], in_=cT[:, :, c0:c0 + n], func=AF.Square), [R_cT], [R_sqt])
                            a = next_acc()
                            P.op("pe", [lambda e, rc=rc, n=n, a=a: e.matmul(
                                out=acc[a][:, :n], lhsT=ones_f[:], rhs=sqt[:, rc, :n],
                                start=(rc == 0), stop=(rc == 3)) for rc in range(4)], [R_sqt, R_const], [R_acc[a]])
                            P.op("act", lambda e, n=n, a=a: e.activation(
                                out=rtt[:, :n], in_=acc[a][:, :n], func=AF.Sqrt, scale=1.0 / 512, bias=EPS),
                                [R_acc[a]], [R_rtt])
                            P.op("dve", lambda e, n=n: e.reciprocal(out=rstdbc[:, :n], in_=rtt[:, :n]),
                                 [R_rtt], [R_rbc])
                            for rc in range(4):
                                P.op("dve", lambda e, rc=rc, c0=c0, n=n: e.scalar_tensor_tensor(
                                    out=dstT[:, rc, c0:c0 + n], in0=cT[:, rc, c0:c0 + n],
                                    scalar=gcol[:, gofs + rc:gofs + rc + 1],
                                    in1=rstdbc[:, :n], op0=ALU.mult, op1=ALU.mult), [R_cT, R_gcol, R_rbc], [R_dst])

                    load_group(0)
                    for gi, (kind, wap, n, blocks, j) in enumerate(groups):
                        if gi + 1 < len(groups):
                            load_group(gi + 1)
                        s = gi % 2
                        if kind in ("ga", "gb", "kr"):
                            next(norm_it[0], None)
                        if kind in ("naq", "nak", "ga", "gb"):
                            func = AF.Sigmoid if kind in ("ga", "gb") else AF.Copy
                            dst = {"naq": s_naq, "nak": s_nak, "ga": s_ga, "gb": s_gb}[kind]
                            ntok = S if blocks is QB else T
                            o = cnt["outt"] % 2
                            cnt["outt"] += 1
                            for (c0, nn) in blocks:
                                a = next_acc()
                                proj_fm(s, 0, 128, c0, nn, a)
                                P.op("act", lambda e, o=o, a=a, c0=c0, nn=nn, func=func: e.activation(
                                    out=outt[o][:, c0:c0 + nn], in_=acc[a][:, :nn], func=func),
                                    [R_acc[a]], [R_outt[o]])
                            P.dma("act", dst[j], outt[o][:, :ntok], [R_outt[o]], [R_scr[kind]], f"outt{o}")
                        elif kind in ("cq", "ckv"):
                            rc = j
                            for (c0, nn) in blocks:
                                a = next_acc()
                                proj_fm(s, 0, 128, c0, nn, a)
                                P.op("act", lambda e, a=a, c0=c0, nn=nn, rc=rc: e.copy(
                                    out=cT[:, rc, c0:c0 + nn], in_=acc[a][:, :nn]), [R_acc[a]], [R_cT])
                            if j == 3:
                                for _ in norm_it[0]:
                                    pass
                                if kind == "cq":
                                    norm_it[0] = c_norm(S, QB, 0, cqnT, R_cqn)
                                else:
                                    norm_it[0] = c_norm(T, TB, 4, ckvnT, R_ckvn)
                        elif kind == "kr":
                            for (c0, nn) in blocks:
                                a = next_acc()
                                b = next_acc()
                                proj_fm(s, 0, 64, c0, nn, a)
                                proj_fm(s, 64, 64, c0, nn, b)
                                P.op("dve", lambda e, a=a, c0=c0, nn=nn: e.tensor_tensor(
                                    out=t1[:, :nn], in0=acc[a][:64, :nn], in1=ropeC[:, c0:c0 + nn], op=ALU.mult),
                                    [R_acc[a], R_rope], [R_t1])
                                P.op("dve", lambda e, b=b, c0=c0, nn=nn: e.tensor_tensor(
                                    out=t2[:, :nn], in0=acc[b][:64, :nn], in1=ropeS[:, c0:c0 + nn], op=ALU.mult),
                                    [R_acc[b], R_rope], [R_t2])
                                P.op("dve", lambda e, c0=c0, nn=nn: e.tensor_tensor(
                                    out=kropeT[0:64, c0:c0 + nn], in0=t1[:, :nn], in1=t2[:, :nn], op=ALU.add),
                                    [R_t1, R_t2], [R_krope])
                        elif kind == "nav":
                            o = cnt["outt"] % 2
                            cnt["outt"] += 1
                            for (c0, nn) in blocks_nav:
                                a = next_acc()
                                proj_fm(s, 0, 128, c0, nn, a)
                                P.op("act", lambda e, o=o, a=a, c0=c0, nn=nn: e.copy(
                                    out=outt[o][:, c0:c0 + nn], in_=acc[a][:, :nn]), [R_acc[a]], [R_outt[o]])
                            for g4 in range(5):
                                tiles = list(range(4 * g4, min(4 * g4 + 4, 17)))
                                v_ = cnt["vout"] % 2
                                cnt["vout"] += 1
                                p_ = cnt["vout"] % 2
                                fns = []
                                for jj, ti in enumerate(tiles):
                                    rows = 128 if ti < 16 else NM
                                    fns.append(lambda e, jj=jj, ti=ti, rows=rows, o=o, p_=p_: e.transpose(
                                        out=pTv[p_][:rows, jj * 128:(jj + 1) * 128], in_=outt[o][:, ti * 128:ti * 128 + rows],
                                        identity=ident[:]))
                                P.op("pe", fns, [R_outt[o], R_const], [R_pTv[p_]])
                                if g4 < 4:
                                    P.op("act", lambda e, v_=v_, p_=p_: e.copy(
                                        out=vout[v_][:].rearrange("p t c -> p (t c)"), in_=pTv[p_][:, :]),
                                        [R_pTv[p_]], [R_vout[v_]])
                                    P.dma("act", s_nav[g4 * 512:(g4 + 1) * 512, j * 128:(j + 1) * 128].rearrange(
                                        "(t p) c -> p t c", p=128), vout[v_][:], [R_vout[v_]], [R_scr["nav"]], f"vout{v_}")
                                else:
                                    P.op("act", lambda e, v_=v_, p_=p_: e.copy(out=vout[v_][:NM, 0, :], in_=pTv[p_][:NM, 0:128]),
                                         [R_pTv[p_]], [R_vout[v_]])
                                    P.dma("act", s_nav[S:T, j * 128:(j + 1) * 128], vout[v_][:NM, 0, :],
                                          [R_vout[v_]], [R_scr["nav"]], f"vout{v_}")
                    for _ in norm_it[0]:
                        pass
                    if d_cqn is not None:
                        P.dma("sp", d_cqn, cqnT[:], [R_cqn], [R_scr["dbg"]], "dbg")
                    if d_ckvn is not None:
                        P.dma("sp", d_ckvn, ckvnT[:], [R_ckvn], [R_scr["dbg"]], "dbg")
                    if d_krope is not None:
                        P.dma("sp", d_krope, kropeT[:], [R_krope], [R_scr["dbg"]], "dbg")
                    done()
            if stop_after == "B":
                return nc
            with ExitStack() as st:
                pc_alloc(st, "C")
                wst = [sb(st, f"cwst{i}", [128, 8, 256], F32) for i in range(2)]
                wbf = [sb(st, f"cwbf{i}", [128, 8, 256], BF16) for i in range(2)]
                qnT = [sb(st, f"qnT{i}", [128, S], BF16) for i in range(2)]
                qrT = [sb(st, f"qrT{i}", [128, S], BF16) for i in range(2)]
                knT = [sb(st, f"knT{i}", [128, T], BF16) for i in range(2)]
                vtm = [sb(st, f"vtm{i}", [128, 17, 128], BF16) for i in range(2)]
                oT = [sb(st, f"oT{i}", [128, S], BF16) for i in range(2)]
                ptl = [sb(st, f"ptl{i}", [128, 512], BF16) for i in range(3)]
                rct = sb(st, "crct", [128, 512], F32)
                t1 = sb(st, "ct1", [64, 512], F32)
                t2 = sb(st, "ct2", [64, 512], F32)
                rxa = sb(st, "crxa", [64, 512], F32)
                rxb = sb(st, "crxb", [64, 512], F32)
                R_rxa, R_rxb = Res(), Res()
                pa = [ps(st, f"cpa{i}", [128, 512], F32) for i in range(2)]
                pS = [ps(st, f"cpS{i}", [128, 512], F32) for i in range(2)]
                pO = [ps(st, f"cpO{i}", [128, 512], F32) for i in range(2)]
                pSum = [ps(st, f"cpSum{i}", [128, 512], F32) for i in range(2)]
                R_wst, R_wbf, R_qn, R_qr, R_kn, R_v, R_oT, R_pa, R_pS, R_pO = ([Res(), Res()] for _ in range(10))
                R_pt = [Res() for _ in range(3)]
                R_rct, R_t1, R_t2 = Res(), Res(), Res()
                for i in range(2):
                    P.op("dve", lambda e, i=i: e.memset(qrT[i][:], 0.0), [], [R_qr[i]])
                cc = {"pa": 0}

                def next_pa():
                    a = cc["pa"] % 2
                    cc["pa"] += 1
                    return a

                def load_w(h):
                    s = h % 2
                    P.dma("sp", wst[s][:, 0:4, :], w_uqh[h].rearrange("(c p) f -> p c f", p=128), [], [R_wst[s]], f"cwst{s}")
                    P.dma("sp", wst[s][:, 4:8, :], w_ukv[:, h * 256:(h + 1) * 256].rearrange("(c p) f -> p c f", p=128),
                          [], [R_wst[s]], f"cwst{s}")
                    P.op("pool", lambda e, s=s: e.tensor_copy(out=wbf[s][:], in_=wst[s][:]), [R_wst[s]], [R_wbf[s]])

                def project(h):
                    s = h % 2

                    def qn_blk(c0, n):
                        a = next_pa()
                        P.op("pe", [lambda e, rc=rc: e.matmul(
                            out=pa[a][:, :n], lhsT=wbf[s][:, rc, 0:128], rhs=cqnT[:, rc, c0:c0 + n],
                            start=(rc == 0), stop=(rc == 3)) for rc in range(4)], [R_wbf[s], R_cqn], [R_pa[a]])
                        P.op("act", lambda e: e.copy(out=qnT[s][:, c0:c0 + n], in_=pa[a][:, :n]), [R_pa[a]], [R_qn[s]])

                    def rope_blk(c0, n):
                        a = next_pa()
                        P.op("pe", [lambda e, rc=rc: e.matmul(
                            out=pa[a][:64, :n], lhsT=wbf[s][:, rc, 128:192], rhs=cqnT[:, rc, c0:c0 + n],
                            start=(rc == 0), stop=(rc == 3)) for rc in range(4)], [R_wbf[s], R_cqn], [R_pa[a]])
                        P.op("act", lambda e: e.copy(out=rxa[:, :n], in_=pa[a][:64, :n]), [R_pa[a]], [R_rxa])
                        P.op("dve", lambda e: e.tensor_tensor(
                            out=t1[:, :n], in0=rxa[:, :n], in1=ropeC[:, c0:c0 + n], op=ALU.mult),
                            [R_rxa, R_rope], [R_t1])
                        b = next_pa()
                        P.op("pe", [lambda e, rc=rc: e.matmul(
                            out=pa[b][:64, :n], lhsT=wbf[s][:, rc, 192:256], rhs=cqnT[:, rc, c0:c0 + n],
                            start=(rc == 0), stop=(rc == 3)) for rc in range(4)], [R_wbf[s], R_cqn], [R_pa[b]])
                        P.op("act", lambda e: e.copy(out=rxb[:, :n], in_=pa[b][:64, :n]), [R_pa[b]], [R_rxb])
                        P.op("dve", lambda e: e.tensor_tensor(
                            out=t2[:, :n], in0=rxb[:, :n], in1=ropeS[:, c0:c0 + n], op=ALU.mult),
                            [R_rxb, R_rope], [R_t2])
                        P.op("dve", lambda e: e.tensor_tensor(
                            out=qrT[s][0:64, c0:c0 + n], in0=t1[:, :n], in1=t2[:, :n], op=ALU.add),
                            [R_t1, R_t2], [R_qr[s]])

                    def kn_blk(c0, n):
                        a = next_pa()
                        P.op("pe", [lambda e, rc=rc: e.matmul(
                            out=pa[a][:, :n], lhsT=wbf[s][:, 4 + rc, 0:128], rhs=ckvnT[:, rc, c0:c0 + n],
                            start=(rc == 0), stop=(rc == 3)) for rc in range(4)], [R_wbf[s], R_ckvn], [R_pa[a]])
                        P.op("act", lambda e: e.copy(out=knT[s][:, c0:c0 + n], in_=pa[a][:, :n]), [R_pa[a]], [R_kn[s]])

                    def v_grp(g):
                        tiles = list(range(4 * g, min(4 * g + 4, 17)))
                        a = next_pa()
                        fns = []
                        for jj, ti in enumerate(tiles):
                            rows = 128 if ti < 16 else NM
                            for rc in range(4):
                                fns.append(lambda e, rc=rc, jj=jj, ti=ti, rows=rows: e.matmul(
                                    out=pa[a][:rows, jj * 128:(jj + 1) * 128], lhsT=ckvnT[:, rc, ti * 128:ti * 128 + rows],
                                    rhs=wbf[s][:, 4 + rc, 128:256], start=(rc == 0), stop=(rc == 3)))
                        P.op("pe", fns, [R_wbf[s], R_ckvn], [R_pa[a]])
                        if g < 4:
                            P.op("act", lambda e: e.copy(
                                out=vtm[s][:, 4 * g:4 * g + 4, :], in_=pa[a][:, :].rearrange("p (j c) -> p j c", c=128)),
                                [R_pa[a]], [R_v[s]])
                        else:
                            P.op("act", lambda e: e.copy(out=vtm[s][:NM, 16, :], in_=pa[a][:NM, 0:128]),
                                 [R_pa[a]], [R_v[s]])

                    for bi, (c0, n) in enumerate(QB):
                        rope_blk(c0, n)
                        qn_blk(c0, n)
                        kn_blk(c0, n)
                        v_grp(bi)
                    kn_blk(*TB[4])
                    v_grp(4)

                def attention(h):
                    s = h % 2
                    items = [(qb, kt) for qb in range(4) for kt in range(17)]

                    def s_step(idx):
                        qb, kt = items[idx]
                        c0 = qb * 512
                        kr = 128 if kt < 16 else NM
                        k0 = kt * 128
                        p_ = idx % 2
                        P.op("pe", [
                            lambda e: e.matmul(out=pS[p_][:kr, :], lhsT=knT[s][:, k0:k0 + kr], rhs=qnT[s][:, c0:c0 + 512],
                                               start=True, stop=False),
                            lambda e: e.matmul(out=pS[p_][:kr, :], lhsT=kropeT[:, k0:k0 + kr], rhs=qrT[s][:, c0:c0 + 512],
                                               start=False, stop=True)],
                            [R_kn[s], R_qn[s], R_krope, R_qr[s]], [R_pS[p_]])
                        P.op("act", lambda e: e.activation(out=ptl[idx % 3][:kr, :], in_=pS[p_][:kr, :], func=AF.Exp,
                                                           scale=MLA_SCALE), [R_pS[p_]], [R_pt[idx % 3]])

                    def pv_step(idx):
                        qb, kt = items[idx]
                        c0 = qb * 512
                        kr = 128 if kt < 16 else NM
                        ob = qb % 2
                        P.op("pe", [
                            lambda e: e.matmul(out=pO[ob][:, :], lhsT=vtm[s][:kr, kt, :], rhs=ptl[idx % 3][:kr, :],
                                               start=(kt == 0), stop=(kt == 16)),
                            lambda e: e.matmul(out=pSum[ob][:, :], lhsT=ones_bf[:kr, :], rhs=ptl[idx % 3][:kr, :],
                                               start=(kt == 0), stop=(kt == 16))],
                            [R_v[s], R_pt[idx % 3], R_const], [R_pO[ob]])
                        if kt == 16:
                            P.op("dve", lambda e: e.reciprocal(out=rct[:], in_=pSum[ob][:, :]), [R_pO[ob]], [R_rct])
                            P.op("dve", lambda e: e.tensor_tensor(out=oT[s][:, c0:c0 + 512], in0=pO[ob][:, :], in1=rct[:],
                                                                  op=ALU.mult), [R_pO[ob], R_rct], [R_oT[s]])

                    for idx in range(len(items) + 1):
                        if idx < len(items):
                            s_step(idx)
                        if idx >= 1:
                            pv_step(idx - 1)
                    P.dma("sp", s_omla[h], oT[s][:], [R_oT[s]], [R_scr["omla"]], f"oT{s}")

                NH = 16
                load_w(0)
                load_w(1)
                project(0)
                for h in range(NH):
                    if h + 1 < NH:
                        project(h + 1)
                    if h + 2 < NH:
                        load_w(h + 2)
                    pc_advance(10)
                    attention(h)
                done()
        if stop_after == "C":
            return nc

        with ExitStack() as st:
            pc_alloc(st, "D")
            qh = sb(st, "qh", [128, 4, S], BF16)
            kh = sb(st, "kh", [128, 4, T], BF16)
            vh = sb(st, "vh", [128, 17, 512], BF16)
            tI = sb(st, "tI", [128, 5, 1024], F32)
            tBd2 = [sb(st, f"tBd{i}", [128, 4, 1024], F32) for i in range(2)]
            R_tB2 = [Res(), Res()]
            bslot = {0: 0, 1: 1, 14: 0, 15: 1}
            tM = sb(st, "tM", [NM, 1024], F32)
            sbias = [sb(st, f"sbias{i}", [128, 1024], F32) for i in range(2)]
            ptd = [sb(st, f"ptd{i}", [128, 6, 1024], BF16) for i in range(2)]
            onaT = sb(st, "onaT", [128, 4, S], BF16)
            rct = sb(st, "drct", [128, 512], F32)
            pS = [ps(st, f"dpS{i}", [128, 1024], F32) for i in range(2)]
            pO = [ps(st, f"dpO{i}", [128, 512], F32) for i in range(2)]
            pSum = [ps(st, f"dpSum{i}", [128, 512], F32) for i in range(2)]
            R_q, R_k, R_v, R_tI, R_tB, R_tM, R_ona, R_rct = (Res() for _ in range(8))
            R_sb, R_pt, R_pS, R_pO = ([Res(), Res()] for _ in range(4))
            cS = {"n": 0}
            bmap = {0: 0, 1: 1, 14: 2, 15: 3}

            for hp in range(2):
                def load_btab(i, q="sp"):
                    b = bmap[i]
                    P.dma(q, tBd2[bslot[i]][:], tabB[hp, 4 * b:4 * b + 4].rearrange("d k f -> k d f"), [],
                          [R_tB2[bslot[i]]], f"dtB{bslot[i]}")

                P.dma("sp", qh[:], s_naq[4 * hp:4 * hp + 4].rearrange("c p t -> p c t"), [R_scr["naq"]], [R_q], "dq")
                P.dma("act", kh[:], s_nak[4 * hp:4 * hp + 4].rearrange("c p t -> p c t"), [R_scr["nak"]], [R_k], "dk")
                load_btab(0, "pool")
                P.dma("sp", tM[:], tabM[hp], [], [R_tM], "dtM")
                P.dma("act", vh[:, 0:16, :], s_nav[0:S, hp * 512:(hp + 1) * 512].rearrange("(t p) c -> p t c", p=128),
                      [R_scr["nav"]], [R_v], "dv")
                P.dma("act", vh[:NM, 16, :], s_nav[S:T, hp * 512:(hp + 1) * 512], [R_scr["nav"]], [R_v], "dv")
                load_btab(1, "pool")
                P.dma("sp", tI[:], tabI[hp].rearrange("d k f -> k d f"), [], [R_tI], "dtI")

                def tile_plan(i):
                    if i in bmap:
                        k0 = 0 if i < 2 else 12
                        plan = [(k0 + j, 128, tBd2[bslot[i]], j, R_tB2[bslot[i]]) for j in range(4)]
                    else:
                        plan = [(i - 2 + j, 128, tI, j, R_tI) for j in range(5)]
                    plan.append((16, NM, tM, None, R_tM))
                    return plan

                def s_phase(i):
                    pb = i % 2
                    for j, (kt, kr, tab, tj, R_tab) in enumerate(tile_plan(i)):
                        p_ = cS["n"] % 2
                        cS["n"] += 1
                        P.op("pe", [lambda e, e_=e_, pp=pp, kt=kt, kr=kr, p_=p_, i=i: e.matmul(
                            out=pS[p_][:kr, e_ * 512 + pp * 128:e_ * 512 + (pp + 1) * 128],
                            lhsT=kh[64 * e_:64 * e_ + 64, pp, kt * 128:kt * 128 + kr],
                            rhs=qh[64 * e_:64 * e_ + 64, pp, i * 128:(i + 1) * 128], start=True, stop=True)
                            for pp in range(4) for e_ in range(2)], [R_k, R_q], [R_pS[p_]])
                        tab_ap = tab[:kr, tj, :] if tj is not None else tab[:kr, :]
                        P.op("dve", lambda e, p_=p_, kr=kr, tab_ap=tab_ap: e.scalar_tensor_tensor(
                            out=sbias[p_][:kr, :], in0=pS[p_][:kr, :], scalar=NA_SCALE, in1=tab_ap,
                            op0=ALU.mult, op1=ALU.add), [R_pS[p_], R_tab], [R_sb[p_]])
                        P.op("act", lambda e, p_=p_, kr=kr, j=j, pb=pb: e.activation(
                            out=ptd[pb][:kr, j, :], in_=sbias[p_][:kr, :], func=AF.Exp), [R_sb[p_]], [R_pt[pb]])

                def pv_phase(i):
                    pb = i % 2
                    ob = i % 2
                    plan = tile_plan(i)
                    nj = len(plan)
                    fns = []
                    for pp in range(4):
                        for j, (kt, kr, tab, tj, R_tab) in enumerate(plan):
                            for e_ in range(2):
                                hh = e_ * 4 + pp
                                vc = (2 * pp + e_) * 64
                                fns.append(lambda e, e_=e_, pp=pp, hh=hh, vc=vc, j=j, kt=kt, kr=kr: e.matmul(
                                    out=pO[ob][64 * e_:64 * e_ + 64, pp * 128:(pp + 1) * 128],
                                    lhsT=vh[:kr, kt, vc:vc + 64], rhs=ptd[pb][:kr, j, hh * 128:(hh + 1) * 128],
                                    start=(j == 0), stop=(j == nj - 1)))
                    for j, (kt, kr, tab, tj, R_tab) in enumerate(plan):
                        for e_ in range(2):
                            fns.append(lambda e, e_=e_, j=j, kr=kr: e.matmul(
                                out=pSum[ob][64 * e_:64 * e_ + 64, :], lhsT=ones_bf[:kr, 0:64],
                                rhs=ptd[pb][:kr, j, e_ * 512:(e_ + 1) * 512], start=(j == 0), stop=(j == nj - 1)))
                    P.op("pe", fns, [R_v, R_pt[pb], R_const], [R_pO[ob]])

                def pv_epi(i):
                    ob = i % 2
                    P.op("dve", lambda e: e.reciprocal(out=rct[:], in_=pSum[ob][:, :]), [R_pO[ob]], [R_rct])
                    P.op("dve", lambda e: e.tensor_tensor(
                        out=onaT[:, :, i * 128:(i + 1) * 128], in0=pO[ob][:, :].rearrange("p (c q) -> p c q", q=128),
                        in1=rct[:, :].rearrange("p (c q) -> p c q", q=128), op=ALU.mult), [R_pO[ob], R_rct], [R_ona])

                for i in range(18):
                    if i < 16:
                        s_phase(i)
                    if i == 1:
                        load_btab(14)
                        load_btab(15)
                    pc_advance(2)
                    if 2 <= i:
                        pv_epi(i - 2)
                    if 1 <= i <= 16:
                        pv_phase(i - 1)
                P.dma("sp", s_ona[4 * hp:4 * hp + 4].rearrange("c p t -> p c t"), onaT[:], [R_ona], [R_scr["ona"]], "dona")
            done()
        if stop_after == "D":
            return nc

        with ExitStack() as st:
            pc_alloc(st, "E")
            onaT = sb(st, "e_onaT", [128, 8, S], BF16)
            omlaT = sb(st, "e_omlaT", [128, 16, S], BF16)
            wst = [sb(st, f"ewst{i}", [128, 24, 128], F32) for i in range(2)]
            wbf = [sb(st, f"ewbf{i}", [128, 24, 128], BF16) for i in range(2)]
            gat = [sb(st, f"egat{i}", [128, 2, S], BF16) for i in range(2)]
            mrg = [sb(st, f"emrg{i}", [128, S], BF16) for i in range(2)]
            t1 = sb(st, "et1", [128, 512], F32)
            t2 = sb(st, "et2", [128, 512], F32)
            pA = [ps(st, f"epA{i}", [128, 512], F32) for i in range(2)]
            pB = [ps(st, f"epB{i}", [128, 512], F32) for i in range(2)]
            R_t1, R_t2 = Res(), Res()
            R_wst, R_wbf, R_gat, R_mrg, R_pA, R_pB = ([Res(), Res()] for _ in range(6))
            R_ona2 = [Res() for _ in range(4)]
            R_omla2 = [Res() for _ in range(4)]

            def load_o(qb):
                c0 = qb * 512
                P.dma("sp", onaT[:, :, c0:c0 + 512], s_ona[:, :, c0:c0 + 512].rearrange("c p t -> p c t"),
                      [R_scr["ona"]], [R_ona2[qb]], f"eona{qb}")
                P.dma("act", omlaT[:, :, c0:c0 + 512], s_omla[:, :, c0:c0 + 512].rearrange("c p t -> p c t"),
                      [R_scr["omla"]], [R_omla2[qb]], f"eomla{qb}")

            def load_e(fo):
                s = fo % 2
                P.dma("sp", wst[s][:, 0:8, :], w_na[:, fo * 128:(fo + 1) * 128].rearrange("(c p) f -> p c f", p=128),
                      [], [R_wst[s]], f"ewst{s}")
                P.dma("sp", wst[s][:, 8:24, :], w_mla[:, fo * 128:(fo + 1) * 128].rearrange("(c p) f -> p c f", p=128),
                      [], [R_wst[s]], f"ewst{s}")
                P.dma("act", gat[s][:, 0, :], s_ga[fo], [R_scr["ga"]], [R_gat[s]], f"egat{s}")
                P.dma("act", gat[s][:, 1, :], s_gb[fo], [R_scr["gb"]], [R_gat[s]], f"egat{s}")
                P.op("act", lambda e, s=s: e.copy(out=wbf[s][:], in_=wst[s][:]), [R_wst[s]], [R_wbf[s]])

            load_e(0)
            load_o(0)
            for qb_ in range(1, 4):
                load_o(qb_)
            k = 0
            for fo in range(16):
                if fo + 1 < 16:
                    load_e(fo + 1)
                pc_advance(2)
                s = fo % 2
                for (c0, n) in QB:
                    p_ = k % 2
                    k += 1
                    P.op("pe", [lambda e, c=c, p_=p_, c0=c0, s=s: e.matmul(
                        out=pA[p_][:, :], lhsT=wbf[s][:, c, :], rhs=onaT[:, c, c0:c0 + 512],
                        start=(c == 0), stop=(c == 7)) for c in range(8)], [R_wbf[s], R_ona2[c0 // 512]], [R_pA[p_]])
                    P.op("pe", [lambda e, c=c, p_=p_, c0=c0, s=s: e.matmul(
                        out=pB[p_][:, :], lhsT=wbf[s][:, 8 + c, :], rhs=omlaT[:, c, c0:c0 + 512],
                        start=(c == 0), stop=(c == 15)) for c in range(16)], [R_wbf[s], R_omla2[c0 // 512]], [R_pB[p_]])
                    P.op("dve", lambda e, p_=p_, c0=c0, s=s: e.tensor_tensor(
                        out=t1[:], in0=pA[p_][:, :], in1=gat[s][:, 0, c0:c0 + 512], op=ALU.mult),
                        [R_pA[p_], R_gat[s]], [R_t1])
                    P.op("dve", lambda e, p_=p_, c0=c0, s=s: e.tensor_tensor(
                        out=t2[:], in0=pB[p_][:, :], in1=gat[s][:, 1, c0:c0 + 512], op=ALU.mult),
                        [R_pB[p_], R_gat[s]], [R_t2])
                    P.op("dve", lambda e, c0=c0, s=s: e.tensor_tensor(
                        out=mrg[s][:, c0:c0 + 512], in0=t1[:], in1=t2[:], op=ALU.add), [R_t1, R_t2], [R_mrg[s]])
                P.dma("sp", s_merged[fo], mrg[s][:], [R_mrg[s]], [R_scr["merged"]], f"emrg{s}")
            pc_advance(10 ** 6)
            done()
        if stop_after == "E":
            return nc

        with ExitStack() as st:
            mT = sb(st, "f_mT", [128, 16, S], BF16)
            wo = sb(st, "f_wo", [128, 16, D], BF16)
            wst = [sb(st, f"fwst{i}", [128, 16, 128], F32) for i in range(2)]
            xt = [sb(st, f"fxt{i}", [128, D], F32) for i in range(2)]
            h2t = [sb(st, f"fh2{i}", [128, D], F32) for i in range(2)]
            pa = [ps(st, f"fpa{i}", [128, 512], F32) for i in range(8)]
            R_mT = Res()
            R_wo = [Res() for _ in range(4)]
            R_wst, R_xt, R_h2 = ([Res(), Res()] for _ in range(3))
            R_pa = [Res() for _ in range(8)]
            def load_wo(g):
                s = g % 2
                P.dma("sp" if s == 0 else "act", wst[s][:], w_out[:, g * 128:(g + 1) * 128].rearrange("(c p) f -> p c f", p=128),
                      [], [R_wst[s]], f"fwst{s}")
                P.op("dve", lambda e, s=s, g=g: e.tensor_copy(out=wo[:, :, g * 128:(g + 1) * 128], in_=wst[s][:]),
                     [R_wst[s]], [R_wo[g // 4]])

            load_wo(0)
            R_mTb = [Res() for _ in range(4)]
            for gg in range(4):
                P.dma("pool", mT[:, :, gg * 512:(gg + 1) * 512], s_merged[:, :, gg * 512:(gg + 1) * 512].rearrange("c p t -> p c t"),
                      [R_scr["merged"]], [R_mTb[gg]], f"fmT{gg}")
            for g in range(1, 4):
                load_wo(g)
            for i in range(16):
                s = i % 2
                P.dma("sp", xt[s][:], x[i * 128:(i + 1) * 128, :], [], [R_xt[s]], f"fxt{s}")
                for fb in range(4):
                    a = s * 4 + fb
                    if i == 0 and fb < 3:
                        for g in range(4 * fb + 4, 4 * fb + 8):
                            load_wo(g)
                    P.op("pe", [lambda e, c=c, a=a, i=i, fb=fb: e.matmul(
                        out=pa[a][:, :], lhsT=mT[:, c, i * 128:(i + 1) * 128], rhs=wo[:, c, fb * 512:(fb + 1) * 512],
                        start=(c == 0), stop=(c == 15)) for c in range(16)], [R_mTb[i // 4], R_wo[fb]], [R_pa[a]])
                    P.op("dve", lambda e, a=a, s=s, fb=fb: e.tensor_tensor(
                        out=h2t[s][:, fb * 512:(fb + 1) * 512], in0=pa[a][:, :], in1=xt[s][:, fb * 512:(fb + 1) * 512],
                        op=ALU.add), [R_pa[a], R_xt[s]], [R_h2[s]])
                P.dma("sp", s_h2[i * 128:(i + 1) * 128, :], h2t[s][:], [R_h2[s]], [R_scr["h2"]], f"fh2{s}")
            done()
        if stop_after == "F":
            return nc
        with ExitStack() as st:
            Wp = sb(st, "g_Wp", [128, 16, D], BF16)
            g2bc = sb(st, "g_g2bc", [128, D], F32)
            pcst = sb(st, "g_pcst", [128, 33], F32)
            R_Wp, R_gc = Res(), Res()
            P.dma("sp", g2bc[:], g2.partition_broadcast(128), [], [R_gc], "gc")
            P.dma("sp", pcst[:], peer_c[:, 0:33], [], [R_gc], "gc")
            iota16 = pcst[:, 0:16]
            thr17 = pcst[:, 16:33]
            with ExitStack() as st2:
                wqs = [sb(st2, f"g_wqs{i}", [128, D], F32) for i in range(2)]
                wqb = [sb(st2, f"g_wqb{i}", [128, D], BF16) for i in range(2)]
                sks = sb(st2, "g_sks", [128, 16, 128], F32)
                skb = sb(st2, "g_skb", [128, 16, 128], BF16)
                pw = [ps(st2, f"g_pw{i}", [128, 512], F32) for i in range(2)]
                R_wqs, R_wqb, R_pw = ([Res(), Res()] for _ in range(3))
                R_sk = Res()
                P.dma("sp", sks[:], subkT.rearrange("h c n -> c h n"), [], [R_sk], "gsk")
                P.op("dve", lambda e: e.tensor_copy(out=skb[:], in_=sks[:]), [R_sk], [R_sk])
                k = 0
                for hs in range(16):
                    s = hs % 2
                    P.dma("sp", wqs[s][:], w_qT[hs], [], [R_wqs[s]], f"gwq{s}")
                    P.op("dve", lambda e, s=s: e.tensor_copy(out=wqb[s][:], in_=wqs[s][:]), [R_wqs[s]], [R_wqb[s]])
                    for g in range(4):
                        p_ = k % 2
                        k += 1
                        P.op("pe", [lambda e, jj=jj, g=g, s=s, hs=hs, p_=p_: e.matmul(
                            out=pw[p_][:, jj * 128:(jj + 1) * 128], lhsT=wqb[s][:, (4 * g + jj) * 128:(4 * g + jj + 1) * 128],
                            rhs=skb[:, hs, :], start=True, stop=True) for jj in range(4)], [R_wqb[s], R_sk], [R_pw[p_]])
                        P.op("act", lambda e, g=g, hs=hs, p_=p_: e.copy(
                            out=Wp[:, 4 * g:4 * g + 4, hs * 128:(hs + 1) * 128],
                            in_=pw[p_][:, :].rearrange("p (j c) -> p j c", c=128)), [R_pw[p_]], [R_Wp])
                done()

            pT = ps(st, "g_pT", [128, 16, 128], BF16)
            psc = [ps(st, f"g_psc{i}", [128, 512], F32) for i in range(4)]
            pTs = ps(st, "g_pTs", [128, 512], F32)
            R_pT, R_psc, R_pTs = Res(), Res(), Res()

            class BS:
                pass

            bsets = []
            for z in range(2):
                B = BS()
                B.h2t = sb(st, f"g_h2t{z}", [128, D], F32)
                B.junk = sb(st, f"g_junk{z}", [128, D], BF16)
                B.hn2T = sb(st, f"g_hn2T{z}", [128, 16, 128], BF16)
                B.sc = sb(st, f"g_sc{z}", [128, 2176], F32)
                B.sc2 = sb(st, f"g_sc2{z}", [128, 2048], F32)
                B.cc = sb(st, f"g_cc{z}", [128, 4352], F32)
                B.cand = B.cc[:, 0:2048].rearrange("p (h c) -> p h c", c=256)
                B.cand2 = B.cc[:, 2048:4096].rearrange("p (h c) -> p h c", c=256)
                B.vals = sb(st, f"g_vals{z}", [128, 16, 16], F32)
                B.idxu = sb(st, f"g_idxu{z}", [128, 16, 16], U32)
                B.idxf = sb(st, f"g_idxf{z}", [128, 16, 16], F32)
                B.ctop = sb(st, f"g_ctop{z}", [128, 8, 16], F32)
                B.cidx = sb(st, f"g_cidx{z}", [128, 8, 16], U32)
                B.cidf = sb(st, f"g_cidf{z}", [128, 8, 16], F32)
                B.sm = [sb(st, f"g_sm{z}_{i}", [128, 8, 16], F32) for i in range(4)]
                B.gi = sb(st, f"g_gi{z}", [128, 3, 128], F32)
                B.slT = sb(st, f"g_slT{z}", [128, 3, 128], F32)
                B.zz = sb(st, f"g_zz{z}", [128, 8], F32)
                B.rz = sb(st, f"g_rz{z}", [128, 8], F32)
                B.ssq = sb(st, f"g_ssq{z}", [128, 1], F32)
                B.rt = sb(st, f"g_rt{z}", [128, 1], F32)
                B.rstd = sb(st, f"g_rstd{z}", [128, 1], F32)
                (B.R_h2t, B.R_hn2T, B.R_gi, B.R_slT, B.R_junk, B.R_sc, B.R_sc2, B.R_cand, B.R_cand2, B.R_vals, B.R_idx,
                 B.R_ctop, B.R_cidx, B.R_sm, B.R_st) = (Res() for _ in range(15))
                B.z = z
                bsets.append(B)

            def tile_ops(i, B):
                z = B.z
                hx, junk, hT, sc, sc2, cand, cand2 = B.h2t, B.junk, B.hn2T, B.sc, B.sc2, B.cand, B.cand2
                vals, idxu, idxf, ctop, cidx, cidf = B.vals, B.idxu, B.idxf, B.ctop, B.cidx, B.cidf
                ssq, rt, rstd, zz, rz = B.ssq, B.rt, B.rstd, B.zz, B.rz
                P.dma("sp", hx[:], s_h2[i * 128:(i + 1) * 128, :], [R_scr["h2"]], [B.R_h2t], f"gh2{z}")
                P.op("act", lambda e: e.activation(out=junk[:], in_=hx[:], func=AF.Square, accum_out=ssq[:]),
                     [B.R_h2t], [B.R_junk, B.R_st])
                P.op("act", lambda e: e.activation(out=rt[:], in_=ssq[:], func=AF.Ln, scale=1.0 / D, bias=EPS),
                     [B.R_st], [B.R_st])
                P.op("act", lambda e: e.activation(out=rstd[:], in_=rt[:], func=AF.Exp, scale=-0.5),
                     [B.R_st], [B.R_st])
                yield "FE1"
                P.op("pool", lambda e: e.tensor_scalar(out=hx[:], in0=hx[:], scalar1=rstd[:, 0:1], scalar2=1.0,
                                                       op0=ALU.mult, op1=ALU.mult), [B.R_h2t, B.R_st], [B.R_h2t])
                P.op("pool", lambda e: e.tensor_tensor(out=junk[:], in0=hx[:], in1=g2bc[:], op=ALU.mult),
                     [B.R_h2t, R_gc], [B.R_junk])
                yield
                P.op("pe", [lambda e, dc=dc: e.transpose(out=pT[:, dc, :], in_=junk[:, dc * 128:(dc + 1) * 128],
                                                         identity=ident[:]) for dc in range(16)],
                     [B.R_junk, R_const], [R_pT])
                P.op("act", lambda e: e.copy(out=hT[:], in_=pT[:]), [R_pT], [B.R_hn2T])
                P.dma("sp", s_hn2T[i // 2][:, :, (i % 2) * 128:(i % 2 + 1) * 128], hT[:], [B.R_hn2T], [R_scr["hn2T"]], f"ghT{z}")
                for blk in range(4):
                    P.op("pe", [lambda e, dc=dc, blk=blk: e.matmul(
                        out=psc[blk][:, :], lhsT=hT[:, dc, :], rhs=Wp[:, dc, blk * 512:(blk + 1) * 512],
                        start=(dc == 0), stop=(dc == 15)) for dc in range(16)], [B.R_hn2T, R_Wp], [R_psc])
                for blk in range(4):
                    P.op("act", lambda e, blk=blk: e.copy(out=sc[:, blk * 512:(blk + 1) * 512], in_=psc[blk][:, :]),
                         [R_psc], [B.R_sc])
                yield "FE2"
                for g in range(16):
                    sg = sc[:, g * 128:(g + 1) * 128]
                    sg2 = sc2[:, g * 128:(g + 1) * 128]
                    P.op("dve", lambda e, g=g, sg=sg: e.max(out=vals[:, g, 0:8], in_=sg), [B.R_sc], [B.R_vals])
                    yield
                    P.op("dve", lambda e, g=g, sg=sg: e.max_index(out=idxu[:, g, 0:8], in_max=vals[:, g, 0:8], in_values=sg),
                         [B.R_sc, B.R_vals], [B.R_idx])
                    P.op("dve", lambda e, g=g, sg=sg, sg2=sg2: e.match_replace(
                        out=sg2, in_to_replace=vals[:, g, 0:8], in_values=sg, imm_value=-1e30), [B.R_sc, B.R_vals], [B.R_sc2])
                    yield
                    P.op("dve", lambda e, g=g, sg2=sg2: e.max(out=vals[:, g, 8:16], in_=sg2), [B.R_sc2], [B.R_vals])
                    yield
                    P.op("dve", lambda e, g=g, sg2=sg2: e.max_index(out=idxu[:, g, 8:16], in_max=vals[:, g, 8:16],
                                                                    in_values=sg2), [B.R_sc2, B.R_vals], [B.R_idx])
                    yield
                yield "SUB"
                P.op("dve", lambda e: e.tensor_copy(out=idxf[:], in_=idxu[:]), [B.R_idx], [B.R_idx])
                v4 = vals[:].rearrange("p (h s) k -> p h s k", s=2)
                i4 = idxf[:].rearrange("p (h s) k -> p h s k", s=2)
                P.op("dve", lambda e: e.tensor_tensor(
                    out=cand.rearrange("p h (a b) -> p h a b", b=16),
                    in0=v4[:, :, 0, :].unsqueeze(3).broadcast_to([128, 8, 16, 16]),
                    in1=v4[:, :, 1, :].unsqueeze(2).broadcast_to([128, 8, 16, 16]), op=ALU.add), [B.R_vals], [B.R_cand])
                yield
                for h in range(8):
                    P.op("dve", lambda e, h=h: e.max(out=ctop[:, h, 0:8], in_=cand[:, h, :]), [B.R_cand], [B.R_ctop])
                    yield
                    P.op("dve", lambda e, h=h: e.max_index(out=cidx[:, h, 0:8], in_max=ctop[:, h, 0:8],
                                                           in_values=cand[:, h, :]), [B.R_cand, B.R_ctop], [B.R_cidx])
                    P.op("dve", lambda e, h=h: e.match_replace(out=cand2[:, h, :], in_to_replace=ctop[:, h, 0:8],
                                                               in_values=cand[:, h, :], imm_value=-1e30),
                         [B.R_cand, B.R_ctop], [B.R_cand2])
                    yield
                    P.op("dve", lambda e, h=h: e.max(out=ctop[:, h, 8:16], in_=cand2[:, h, :]), [B.R_cand2], [B.R_ctop])
                    yield
                    P.op("dve", lambda e, h=h: e.max_index(out=cidx[:, h, 8:16], in_max=ctop[:, h, 8:16],
                                                           in_values=cand2[:, h, :]), [B.R_cand2, B.R_ctop], [B.R_cidx])
                    yield
                P.op("dve", lambda e: e.tensor_copy(out=cidf[:], in_=cidx[:]), [B.R_cidx], [B.R_cidx])
                yield "CAND"
                dd, ex, k1f, k2f = B.sm
                gv = B.gi
                gate3 = gv[:, 0, :].rearrange("p (h k) -> p h k", k=16)
                i1f3 = gv[:, 1, :].rearrange("p (h k) -> p h k", k=16)
                i2f3 = gv[:, 2, :].rearrange("p (h k) -> p h k", k=16)
                P.op("dve", lambda e: e.tensor_tensor(out=dd[:], in0=ctop[:], in1=ctop[:, :, 0:1].broadcast_to([128, 8, 16]),
                                                      op=ALU.subtract), [B.R_ctop], [B.R_sm])
                P.op("act", lambda e: e.activation(out=ex[:], in_=dd[:], func=AF.Exp), [B.R_sm], [B.R_sm])
                yield
                ge = B.cc[:, 0:2176].rearrange("p (h j k) -> p h j k", h=8, j=16)
                eq = sc2[:, :].rearrange("p (h j k) -> p h j k", h=8, j=16)
                P.op("dve", lambda e: e.tensor_tensor(
                    out=ge, in0=cidf[:].unsqueeze(3).broadcast_to([128, 8, 16, 17]),
                    in1=thr17.unsqueeze(1).unsqueeze(1).broadcast_to([128, 8, 16, 17]), op=ALU.is_ge),
                    [B.R_cidx, R_gc], [B.R_cand, B.R_cand2])
                yield
                P.op("dve", lambda e: e.tensor_reduce(out=zz[:], in_=ex[:], axis=AX.X, op=ALU.add), [B.R_sm], [B.R_sm])
                yield
                P.op("dve", lambda e: e.tensor_reduce(out=k1f[:], in_=ge[:, :, :, 1:16], axis=AX.X, op=ALU.add),
                     [B.R_cand, B.R_cand2], [B.R_sm])
                yield
                P.op("dve", lambda e: e.reciprocal(out=rz[:], in_=zz[:]), [B.R_sm], [B.R_sm])
                yield
                P.op("dve", lambda e: e.tensor_tensor(out=eq, in0=ge[:, :, :, 0:16], in1=ge[:, :, :, 1:17],
                                                      op=ALU.subtract), [B.R_cand, B.R_cand2], [B.R_sc2])
                yield
                P.op("dve", lambda e: e.tensor_tensor(
                    out=gate3, in0=ex[:], in1=rz[:].unsqueeze(2).broadcast_to([128, 8, 16]), op=ALU.mult),
                    [B.R_sm], [B.R_gi])
                yield
                P.op("dve", lambda e: e.tensor_tensor(
                    out=eq, in0=eq, in1=i4[:, :, 0, :].unsqueeze(2).broadcast_to([128, 8, 16, 16]), op=ALU.mult),
                    [B.R_sc2, B.R_idx], [B.R_sc2])
                yield
                P.op("dve", lambda e: e.scalar_tensor_tensor(out=k2f[:], in0=k1f[:], scalar=-16.0, in1=cidf[:],
                                                             op0=ALU.mult, op1=ALU.add), [B.R_sm, B.R_cidx], [B.R_sm])
                yield
                P.op("dve", lambda e: e.tensor_reduce(out=i1f3, in_=eq, axis=AX.X, op=ALU.add), [B.R_sc2], [B.R_gi])
                yield
                P.op("dve", lambda e: e.tensor_tensor(
                    out=eq, in0=k2f[:].unsqueeze(3).broadcast_to([128, 8, 16, 16]),
                    in1=iota16.unsqueeze(1).unsqueeze(1).broadcast_to([128, 8, 16, 16]), op=ALU.is_equal),
                    [B.R_sm, R_gc], [B.R_sc2])
                yield
                P.op("dve", lambda e: e.tensor_tensor(
                    out=eq, in0=eq, in1=i4[:, :, 1, :].unsqueeze(2).broadcast_to([128, 8, 16, 16]), op=ALU.mult),
                    [B.R_sc2, B.R_idx], [B.R_sc2])
                yield
                P.op("dve", lambda e: e.tensor_reduce(out=i2f3, in_=eq, axis=AX.X, op=ALU.add), [B.R_sc2], [B.R_gi])
                yield
                P.op("pe", [lambda e, k_=k_: e.transpose(out=pTs[:, k_ * 128:(k_ + 1) * 128], in_=gv[:, k_, :],
                                                         identity=ident_f[:]) for k_ in range(3)],
                     [B.R_gi, R_const], [R_pTs])
                P.op("act", lambda e: e.copy(out=B.slT[:].rearrange("p k t -> p (k t)"), in_=pTs[:, 0:384]),
                     [R_pTs], [B.R_slT])
                P.dma("sp", s_slot[i], B.slT[:].rearrange("p k t -> p (k t)"), [B.R_slT], [R_scr["slot"]], f"gsl{z}")
                yield

            def run_until(gen, tag):
                for v in gen:
                    if v == tag:
                        return
                    yield

            def lane(z):
                gens = [tile_ops(i, bsets[z]) for i in range(z, 16, 2)]
                yield from run_until(gens[0], "FE2")
                for k_, cur in enumerate(gens):
                    nxt = gens[k_ + 1] if k_ + 1 < len(gens) else None
                    yield from run_until(cur, "SUB")
                    if nxt is not None:
                        yield from run_until(nxt, "FE1")
                    yield from run_until(cur, "CAND")
                    if nxt is not None:
                        yield from run_until(nxt, "FE2")
                    yield from run_until(cur, "END")

            alive = [lane(0), lane(1)]
            while alive:
                for gen in list(alive):
                    if next(gen, "end") == "end":
                        alive.remove(gen)
            done()
        if stop_after == "G0":
            return nc

        with ExitStack() as st:
            Gb = [sb(st, f"h_G{i}", [128, 128, 256], BF16) for i in range(2)]
            hnb = sb(st, "h_hnb", [128, 16, 256], BF16)
            NU, NV = 4, 7
            ubuf = [sb(st, f"h_ub{i}", [128, 16, 128], BF16) for i in range(NU)]
            vbuf = [sb(st, f"h_vb{i}", [128, 1024], BF16) for i in range(NV)]
            slT = [sb(st, f"h_slT{i}", [128, 3, 128], F32) for i in range(4)]
            LR = [sb(st, f"h_LR{i}", [128, 8, 2, 128], BF16) for i in range(2)]
            io128 = sb(st, "h_io", [128, 128], F32)
            gl = [sb(st, f"h_gl{i}", [128, 256], BF16) for i in range(2)]
            h2t = [sb(st, f"h_h2t{i}", [128, D], F32) for i in range(2)]
            gfbc = sb(st, "h_gfbc", [128, D], F32)
            ssq = sb(st, "h_ssq", [128, 1], F32)
            rt = sb(st, "h_rt", [128, 1], F32)
            rstd = sb(st, "h_rstd", [128, 1], F32)
            pacc = [ps(st, f"h_pacc{i}", [128, 512], F32) for i in range(4)]
            psS = [ps(st, f"h_psS{i}", [128, 512], F32) for i in range(2)]
            pG = [ps(st, f"h_pG{i}", [128, 512], F32) for i in range(2)]
            R_ub = [Res() for _ in range(NU)]
            R_vb = [Res() for _ in range(NV)]
            R_slT = [Res() for _ in range(4)]
            R_LR, R_gl, R_h2t, R_psS, R_pG, R_G = ([Res(), Res()] for _ in range(6))
            R_pacc = [Res() for _ in range(4)]
            R_hnb, R_gc, R_st = (Res() for _ in range(3))
            junk = LR[0][:].rearrange("p a b c -> p (a b c)")
            P.dma("sp", gfbc[:], gf.partition_broadcast(128), [], [R_gc], "hc")
            P.dma("sp", io128[:], peer_c[:, 33:161], [], [R_gc], "hc")
            cn = {"u": 0, "v": 0, "q": 0, "g": 0, "s": 0}

            def gbuild(b):
                G = Gb[b % 2]
                RG = R_G[b % 2]
                for tt in range(2):
                    ti = 2 * b + tt
                    sl = (2 * b + tt) % 4
                    P.dma("pool", slT[sl][:].rearrange("p k t -> p (k t)"), s_slot[ti], [R_scr["slot"]], [R_slT[sl]], f"hsl{sl}")
                yield
                for q in range(32):
                    tt = q // 16
                    sl = (2 * b + tt) % 4
                    t0 = (q % 16) * 8
                    lr = cn["q"] % 2
                    cn["q"] += 1
                    fns = []
                    for t in range(8):
                        fns.append(lambda e, t=t, sl=sl, t0=t0, lr=lr: e.tensor_scalar(
                            out=LR[lr][:, t, 0, :], in0=io128[:], scalar1=slT[sl][:, 1, t0 + t:t0 + t + 1],
                            scalar2=slT[sl][:, 0, t0 + t:t0 + t + 1], op0=ALU.is_equal, op1=ALU.mult))
                        fns.append(lambda e, t=t, sl=sl, t0=t0, lr=lr: e.tensor_scalar(
                            out=LR[lr][:, t, 1, :], in0=io128[:], scalar1=slT[sl][:, 2, t0 + t:t0 + t + 1],
                            scalar2=None, op0=ALU.is_equal))
                    P.op("dve", fns, [R_slT[sl], R_gc], [R_LR[lr]])
                    for g in range(2):
                        pg = cn["g"] % 2
                        cn["g"] += 1
                        P.op("pe", [lambda e, jj=jj, g=g, lr=lr, pg=pg: e.matmul(
                            out=pG[pg][:, jj * 128:(jj + 1) * 128], lhsT=LR[lr][:, 4 * g + jj, 0, :],
                            rhs=LR[lr][:, 4 * g + jj, 1, :], start=True, stop=True) for jj in range(4)],
                            [R_LR[lr]], [R_pG[pg]])
                        c0 = tt * 128 + t0 + 4 * g
                        P.op("act", lambda e, pg=pg, c0=c0, G=G: e.copy(
                            out=G[:, :, c0:c0 + 4], in_=pG[pg][:, :].rearrange("p (t i) -> p i t", i=128)),
                            [R_pG[pg]], [RG])
                    yield

            def load_hnb(b):
                P.dma("pool", hnb[:], s_hn2T[b], [R_scr["hn2T"]], [R_hnb], "hhn")

            pend_epi = []

            rs2 = [sb(st, f"h_rs2{i}", [128, 1], F32) for i in range(2)]
            R_rs2 = [Res(), Res()]

            def epilogue_g1(b):
                for tt in range(2):
                    ti = 2 * b + tt
                    hx = h2t[tt]
                    P.op("act", lambda e, hx=hx: e.activation(out=junk, in_=hx[:], func=AF.Square, accum_out=ssq[:]),
                         [R_h2t[tt]], [R_LR[0], R_st])
                    P.op("act", lambda e: e.activation(out=rt[:], in_=ssq[:], func=AF.Ln, scale=1.0 / D, bias=EPS),
                         [R_st], [R_st])
                    P.op("act", lambda e, tt=tt: e.activation(out=rs2[tt][:], in_=rt[:], func=AF.Exp, scale=-0.5),
                         [R_st], [R_rs2[tt]])
                    P.op("pool", lambda e, hx=hx, tt=tt: e.tensor_scalar(
                        out=hx[:], in0=hx[:], scalar1=rs2[tt][:, 0:1], scalar2=1.0, op0=ALU.mult, op1=ALU.mult),
                        [R_h2t[tt], R_rs2[tt]], [R_h2t[tt]])
                    P.op("pool", lambda e, hx=hx: e.tensor_tensor(out=hx[:], in0=hx[:], in1=gfbc[:], op=ALU.mult),
                         [R_h2t[tt], R_gc], [R_h2t[tt]])
                    P.dma("pool", out_d[ti * 128:(ti + 1) * 128, :], hx[:], [R_h2t[tt]], [Res()], f"hout{tt}")

            for _ in gbuild(0):
                pass
            for b in range(8):
                G = Gb[b % 2]
                RG = R_G[b % 2]
                gnext = gbuild(b + 1) if b + 1 < 8 else iter(())
                if b == 0:
                    load_hnb(0)

                def load_u(i2):
                    r = cn["u"] % NU
                    cn["u"] += 1
                    P.dma("sp", ubuf[r][:].rearrange("p c i -> p (c i)"), s_uT[i2], [R_scr["uT"]], [R_ub[r]], f"hub{r}")
                    return r

                ring = [load_u(i2) for i2 in range(NU - 1)]
                for i2 in range(128):
                    if i2 + NU - 1 < 128:
                        ring.append(load_u(i2 + NU - 1))
                    r = ring[i2]
                    s_ = cn["s"] % 2
                    cn["s"] += 1
                    P.op("pe", [lambda e, dc=dc, r=r, s_=s_: e.matmul(
                        out=psS[s_][:, 0:256], lhsT=ubuf[r][:, dc, :], rhs=hnb[:, dc, :],
                        start=(dc == 0), stop=(dc == 15)) for dc in range(16)], [R_ub[r], R_hnb], [R_psS[s_]])
                    P.op("act", lambda e, s_=s_: e.activation(out=gl[s_][:], in_=psS[s_][:, 0:256],
                                                              func=AF.Gelu_apprx_tanh), [R_psS[s_]], [R_gl[s_]])
                    P.op("dve", lambda e, s_=s_, i2=i2, G=G: e.tensor_tensor(out=G[:, i2, :], in0=gl[s_][:], in1=G[:, i2, :],
                                                                             op=ALU.mult), [R_gl[s_], RG], [RG])
                    if i2 == 3 and pend_epi:
                        epilogue_g1(pend_epi.pop(0))
                if b + 1 < 8:
                    load_hnb(b + 1)
                for tt in range(2):
                    ti = 2 * b + tt
                    P.dma("pool", h2t[tt][:], s_h2[ti * 128:(ti + 1) * 128, :], [R_scr["h2"]], [R_h2t[tt]], f"hh2{tt}")

                def load_v(i2, half):
                    r = cn["v"] % NV
                    cn["v"] += 1
                    P.dma("sp", vbuf[r][:], s_v2[i2][:, half * 1024:(half + 1) * 1024], [R_scr["v2"]], [R_vb[r]], f"hvb{r}")
                    return r

                for half in range(2):
                    ring = [load_v(i2, half) for i2 in range(NV - 1)]
                    for i2 in range(128):
                        if i2 + NV - 1 < 128:
                            ring.append(load_v(i2 + NV - 1, half))
                        r = ring[i2]
                        P.op("pe", [lambda e, tt=tt, db=db, r=r, i2=i2, G=G: e.matmul(
                            out=pacc[tt * 2 + db][:, :], lhsT=G[:, i2, tt * 128:(tt + 1) * 128],
                            rhs=vbuf[r][:, db * 512:(db + 1) * 512], start=(i2 == 0), stop=(i2 == 127))
                            for tt in range(2) for db in range(2)], [R_vb[r], RG], R_pacc)
                        if i2 % 6 == 2:
                            next(gnext, None)
                    for tt in range(2):
                        for db in range(2):
                            c0 = half * 1024 + db * 512
                            P.op("dve", lambda e, tt=tt, db=db, c0=c0: e.tensor_tensor(
                                out=h2t[tt][:, c0:c0 + 512], in0=pacc[tt * 2 + db][:, :], in1=h2t[tt][:, c0:c0 + 512],
                                op=ALU.add), [R_pacc[tt * 2 + db], R_h2t[tt]], [R_h2t[tt]])
                for _ in gnext:
                    pass
                pend_epi.append(b)
            epilogue_g1(pend_epi.pop(0))
            done()
    return nc


def _host_inputs(inp):
    f = lambda a: np.ascontiguousarray(a, dtype=np.float32)
    w_in = inp["w_in"][0]
    kr = w_in[:, 4096:4160]
    shared = {
        "meta": f(inp["meta_tokens"]),
        "g1": f(inp["norm1_g"][0][None]),
        "g2": f(inp["norm2_g"][0][None]),
        "gf": f(inp["final_norm_g"][None]),
        "w_in": f(w_in),
        "w_kr2": f(np.concatenate([kr, kr[:, 32:], kr[:, :32]], axis=1)),
        "gq": f(inp["mla_q_norm_g"][0].reshape(4, 128).T),
        "gkv": f(inp["mla_kv_norm_g"][0].reshape(4, 128).T),
        "w_ukv": f(inp["mla_w_ukv"][0]),
        "ident": np.eye(128, dtype=np.float32),
        "w_na": f(inp["w_na_branch"][0]),
        "w_mla": f(inp["w_mla_branch"][0]),
        "w_out": f(inp["w_out"][0]),
        "puT": f(inp["peer_u"][0].reshape(128, 128, 16, 128).transpose(1, 3, 2, 0).reshape(128, 128, D)),
        "pv2": f(inp["peer_v"][0].reshape(128, 128, D).transpose(1, 0, 2)),
    }
    wq = inp["mla_w_uq"][0].reshape(512, 16, 192)
    shared["w_uqh"] = f(np.concatenate([wq, wq[:, :, 160:], wq[:, :, 128:160]], axis=2).transpose(1, 0, 2))
    pos = np.concatenate([np.arange(NM, T), np.arange(NM)]).astype(np.float32)
    inv_freq = (10000.0 ** (-np.arange(0, 64, 2, dtype=np.float32) / 64)).astype(np.float32)
    ang = pos[None, :] * inv_freq[:, None]
    cos, sin = np.cos(ang).astype(np.float32), np.sin(ang).astype(np.float32)
    shared["ropeCS"] = f(np.stack([np.concatenate([cos, cos], 0), np.concatenate([-sin, sin], 0)]))
    rb = inp["na_rel_bias"][0]
    mb = inp["na_meta_bias"][0]
    rows = 32

    def rs(r):
        return int(np.clip(r - 4, 0, rows - 8))

    def cs(c):
        return np.clip(c - 8, 0, 64 - 16)

    def pattern(i, kt):
        kk = np.arange(128)
        kr_ = 2 * kt + kk // 64
        kc_ = kk % 64
        qr_ = 2 * i + kk // 64
        qc_ = kk % 64
        dr = kr_[:, None] - qr_[None, :]
        dc = kc_[:, None] - qc_[None, :]
        rs_q = np.array([rs(r) for r in qr_])
        cs_q = cs(qc_)
        valid = ((kr_[:, None] >= rs_q[None, :]) & (kr_[:, None] < rs_q[None, :] + 8)
                 & (kc_[:, None] >= cs_q[None, :]) & (kc_[:, None] < cs_q[None, :] + 16))
        ro = np.clip(dr + 7, 0, 14)
        co = np.clip(dc + 15, 0, 30)
        g = rb[:, ro, co]
        g = np.where(valid[None], g, np.float32(NEG)).astype(np.float32)
        return g.transpose(1, 0, 2)

    def halves(p):
        o = []
        for hp in range(2):
            hs = [8 * hp + 2 * pp + e for e in range(2) for pp in range(4)]
            o.append(p[:, hs, :].reshape(p.shape[0], 1024))
        return o

    tI = [halves(pattern(6, 6 + dt)) for dt in range(-2, 3)]
    shared["tabI"] = f(np.stack([np.stack([t[hp] for t in tI]) for hp in range(2)]))
    bl = []
    for i, k0 in ((0, 0), (1, 0), (14, 12), (15, 12)):
        for kt in range(k0, k0 + 4):
            bl.append(halves(pattern(i, kt)))
    shared["tabB"] = f(np.stack([np.stack([t[hp] for t in bl]) for hp in range(2)]))
    pm = np.broadcast_to(mb.T[:, :, None], (16, 16, 128))
    shared["tabM"] = f(np.stack(halves(pm)))
    shared["peer_c"] = f(np.broadcast_to(np.concatenate([np.arange(16), 16 * np.arange(17), np.arange(128)])[None, :], (128, 161)))
    shared["w_qT"] = f(inp["peer_w_q"][0].T.reshape(16, 128, D))
    shared["subkT"] = f(inp["peer_sub_keys"][0].reshape(16, 128, 128).transpose(0, 2, 1))
    return shared


_CACHE = {}


def kernel(**inp):
    shared = _host_inputs(inp)
    if "nc" not in _CACHE:
        _CACHE["nc"] = build()
    nc = _CACHE["nc"]
    in_maps = []
    for b in range(NCORES):
        m = dict(shared)
        m["x"] = np.ascontiguousarray(inp["x"][b], dtype=np.float32)
        in_maps.append(m)
    res = run_bass_kernel_spmd(nc, in_maps, core_ids=list(range(NCORES)))
    return np.stack([np.asarray(r["out"], dtype=np.float32) for r in res.results], axis=0)
```
